# Optimizing a Trainium2 kernel written in Bass

```python
import math
import jax
import jax.numpy as jnp
from jax import lax
import numpy as np

D_MODEL = 2048
BATCH = 1
SEQ = 8192
DEPTH = 2

GRID_W = 64
CTX_LEN = 256

S5_WIDTH = 512
S5_GROUP = 16
S5_GROUPS = S5_WIDTH // S5_GROUP
S5_STATE = 64
N_DIR = 2
FFT_WIDTH = 512
FFT_GROUPS = 4
FFT_GROUP = FFT_WIDTH // FFT_GROUPS
SGU_WIDTH = 512
SGU_HEADS = 4
SGU_HEAD = SGU_WIDTH // SGU_HEADS
CHUNK = 128
CHUNK_ROWS = CHUNK // GRID_W
N_BRANCH = 3
BRANCH_WIDTH = 512
IN_WIDTH = S5_WIDTH + FFT_WIDTH + 2 * SGU_WIDTH
N_EXPERTS = 32
TOP_K = 4
D_FF = D_MODEL
SWIGLU_LIMIT = 7.0
SWIGLU_ALPHA = 1.702
MOE_BLOCK = 128
N_MOD = 6
EPS = 1e-6

kernel_name = 'hybrid_s5_fnet_sgu_moe_dit'


def rmsnorm(x, g):
    xf = x.astype(jnp.float32)
    y = xf * lax.rsqrt(jnp.mean(xf * xf, axis=-1, keepdims=True) + EPS)
    return (y * g.astype(jnp.float32)).astype(x.dtype)


def adaln(cond, w_mod, b_mod):
    m = jax.nn.silu(cond) @ w_mod + b_mod
    return jnp.split(m, N_MOD, axis=-1)


def modulate(h, shift, scale):
    return h * (1.0 + scale) + shift


def s5_discretize(a_re, a_im, log_dt, b_re, b_im):
    lam = lax.complex(a_re.astype(jnp.float32), a_im.astype(jnp.float32))
    dt = jnp.exp(log_dt.astype(jnp.float32))[..., None]
    a_bar = jnp.exp(lam * dt)
    b = lax.complex(b_re.astype(jnp.float32), b_im.astype(jnp.float32))
    b_bar = ((a_bar - 1.0) / lam)[..., None] * b
    return a_bar, b_bar


def _ssm_combine(left, right):
    a_l, b_l = left
    a_r, b_r = right
    return a_r * a_l, a_r * b_l + b_r


def s5_scan(u, a_bar, b_bar, h0):
    u_dir = jnp.stack([u, jnp.flip(u, axis=1)]).astype(jnp.float32).astype(jnp.complex64)
    bu = jnp.einsum('dblgh,dgph->dblgp', u_dir, b_bar)
    bu = bu.at[:, :, 0].add(a_bar[:, None] * h0)
    a = jnp.broadcast_to(a_bar[:, None, None], bu.shape)
    _, states = lax.associative_scan(_ssm_combine, (a, bu), axis=2)
    return states


def s5_readout(states, c_re, c_im, d_skip, u):
    c = lax.complex(c_re.astype(jnp.float32), c_im.astype(jnp.float32))
    y = jnp.real(jnp.einsum('dblgp,dghp->dblgh', states, c))
    y = y[0] + jnp.flip(y[1], axis=1)
    return y + d_skip.astype(jnp.float32).reshape(S5_GROUPS, S5_GROUP) * u.astype(jnp.float32)


def fourier_mix(z):
    bsz, n, _ = z.shape
    zg = z.astype(jnp.float32).reshape(bsz, n, FFT_GROUPS, FFT_GROUP).transpose(0, 2, 1, 3)
    f = jnp.real(jnp.fft.fft2(zg, norm='ortho'))
    return f.transpose(0, 2, 1, 3).reshape(bsz, n, FFT_WIDTH).astype(z.dtype)


def spatial_gating(z, g_v, w_s, b_s, n_chunks):
    u, v = jnp.split(z, 2, axis=-1)
    v = rmsnorm(v, g_v)
    bsz, n, _ = v.shape
    vc = v.reshape(bsz, n_chunks, CHUNK, SGU_HEADS, SGU_HEAD)
    s = jnp.einsum('hqk,bnkhc->bnqhc', w_s, vc) + b_s.T[None, None, :, :, None]
    return u * s.reshape(bsz, n, SGU_WIDTH)


def branch_features(z, states, c_re, c_im, d_skip, w_glu, g_v, w_s, b_s, n_chunks):
    bsz, n, _ = z.shape
    z_s5 = z[..., :S5_WIDTH]
    z_fft = z[..., S5_WIDTH:S5_WIDTH + FFT_WIDTH]
    z_sgu = z[..., S5_WIDTH + FFT_WIDTH:]
    y = s5_readout(states, c_re, c_im, d_skip, z_s5.reshape(bsz, n, S5_GROUPS, S5_GROUP))
    y = jax.nn.gelu(y.reshape(bsz, n, S5_WIDTH)).astype(z.dtype)
    y_s5 = y * jax.nn.sigmoid(y @ w_glu)
    y_fft = fourier_mix(z_fft)
    y_sgu = spatial_gating(jax.nn.gelu(z_sgu), g_v, w_s, b_s, n_chunks)
    return jnp.stack([y_s5, y_fft, y_sgu], axis=2)


def gated_merge(h, feats, w_branch, w_gate, b_gate, w_out):
    bsz, n, _ = h.shape
    br = jnp.einsum('blkf,kfd->blkd', feats, w_branch)
    gates = jax.nn.sigmoid(h @ w_gate + b_gate).reshape(bsz, n, N_BRANCH, D_MODEL)
    return jnp.einsum('blkd,blkd->bld', gates, br) @ w_out


def moe_ffn(h, router_w, router_b, w_up, b_up, w_down, b_down):
    n = h.shape[0]
    logits = (h @ router_w).astype(jnp.float32) + router_b.astype(jnp.float32)
    top_val, top_idx = lax.top_k(logits, TOP_K)
    top_w = jax.nn.softmax(top_val, axis=-1)
    n_assign = n * TOP_K
    flat_e = top_idx.reshape(-1)
    flat_tok = jnp.repeat(jnp.arange(n, dtype=jnp.int32), TOP_K)
    flat_w = top_w.reshape(-1)
    order = jnp.argsort(flat_e)
    e_sorted = flat_e[order]
    tok_sorted = flat_tok[order]
    w_sorted = flat_w[order]
    counts = jnp.bincount(flat_e, length=N_EXPERTS)
    padded = (counts + MOE_BLOCK - 1) // MOE_BLOCK * MOE_BLOCK
    start = jnp.cumsum(counts) - counts
    pend = jnp.cumsum(padded)
    pstart = pend - padded
    dest = pstart[e_sorted] + (jnp.arange(n_assign) - start[e_sorted])
    n_rows = (n_assign + MOE_BLOCK - 1) // MOE_BLOCK * MOE_BLOCK + N_EXPERTS * MOE_BLOCK
    n_blocks = n_rows // MOE_BLOCK
    row_tok = jnp.zeros((n_rows,), jnp.int32).at[dest].set(tok_sorted)
    row_w = jnp.zeros((n_rows,), jnp.float32).at[dest].set(w_sorted)
    block_e = jnp.searchsorted(pend, jnp.arange(n_blocks) * MOE_BLOCK, side='right')
    block_e = jnp.minimum(block_e, N_EXPERTS - 1)

    def expert_block(args):
        tok, w, e = args
        xb = h[tok]
        gu = xb @ w_up[e] + b_up[e]
        gate, up = jnp.split(gu, 2, axis=-1)
        gate = jnp.minimum(gate, SWIGLU_LIMIT)
        up = jnp.clip(up, -SWIGLU_LIMIT, SWIGLU_LIMIT)
        act = gate * jax.nn.sigmoid(SWIGLU_ALPHA * gate) * (up + 1.0)
        yb = act @ w_down[e] + b_down[e]
        return yb * w[:, None].astype(yb.dtype)

    rows = lax.map(expert_block, (row_tok.reshape(n_blocks, MOE_BLOCK),
                                  row_w.reshape(n_blocks, MOE_BLOCK), block_e))
    return jnp.zeros_like(h).at[row_tok].add(rows.reshape(n_rows, D_MODEL))


def setup_inputs(seed: int = 0) -> dict:
    key = jax.random.key(seed)
    ks = jax.random.split(key, 32)
    f32 = jnp.float32

    def nrm(k, shape, scale):
        return jax.random.normal(k, shape, f32) * scale

    L = DEPTH
    n_idx = jnp.arange(S5_STATE, dtype=f32)
    s5_shape = (L, N_DIR, S5_GROUPS, S5_STATE)
    return {
        'x': nrm(ks[0], (BATCH, SEQ, D_MODEL), 1.0),
        'c': nrm(ks[1], (BATCH, D_MODEL), 1.0),
        'ctx': nrm(ks[2], (BATCH, CTX_LEN, D_MODEL), 1.0),
        'c_ctx': nrm(ks[3], (D_MODEL,), 1.0),
        'w_mod': nrm(ks[4], (L, D_MODEL, N_MOD * D_MODEL), 0.5 * D_MODEL ** -0.5),
        'b_mod': nrm(ks[5], (L, N_MOD * D_MODEL), 0.01),
        'norm1_g': 1.0 + nrm(ks[6], (L, D_MODEL), 0.1),
        'norm2_g': 1.0 + nrm(ks[7], (L, D_MODEL), 0.1),
        'w_in': nrm(ks[8], (L, D_MODEL, IN_WIDTH), D_MODEL ** -0.5),
        's5_a_re': -0.5 + nrm(ks[9], s5_shape, 0.02),
        's5_a_im': math.pi * n_idx + nrm(ks[10], s5_shape, 0.02),
        's5_log_dt': jax.random.uniform(ks[11], (L, N_DIR, S5_GROUPS), f32,
                                        math.log(1e-3), math.log(1e-1)),
        's5_b_re': nrm(ks[12], (L, N_DIR, S5_GROUPS, S5_STATE, S5_GROUP), (2 * S5_GROUP) ** -0.5),
        's5_b_im': nrm(ks[13], (L, N_DIR, S5_GROUPS, S5_STATE, S5_GROUP), (2 * S5_GROUP) ** -0.5),
        's5_c_re': nrm(ks[14], (L, N_DIR, S5_GROUPS, S5_GROUP, S5_STATE), 0.5),
        's5_c_im': nrm(ks[15], (L, N_DIR, S5_GROUPS, S5_GROUP, S5_STATE), 0.5),
        's5_d': nrm(ks[16], (L, S5_WIDTH), 0.5),
        's5_w_glu': nrm(ks[17], (L, S5_WIDTH, S5_WIDTH), S5_WIDTH ** -0.5),
        'sgu_norm_g': 1.0 + nrm(ks[18], (L, SGU_WIDTH), 0.1),
        'sgu_w': nrm(ks[19], (L, SGU_HEADS, CHUNK, CHUNK), 0.5 * CHUNK ** -0.5),
        'sgu_b': 1.0 + nrm(ks[20], (L, SGU_HEADS, CHUNK), 0.1),
        'w_branch': nrm(ks[21], (L, N_BRANCH, BRANCH_WIDTH, D_MODEL), BRANCH_WIDTH ** -0.5),
        'w_gate': nrm(ks[22], (L, D_MODEL, N_BRANCH * D_MODEL), D_MODEL ** -0.5),
        'b_gate': nrm(ks[23], (L, N_BRANCH * D_MODEL), 0.1),
        'w_out': nrm(ks[24], (L, D_MODEL, D_MODEL), D_MODEL ** -0.5),
        'router_w': nrm(ks[25], (L, D_MODEL, N_EXPERTS), D_MODEL ** -0.5),
        'router_b': nrm(ks[26], (L, N_EXPERTS), 0.01),
        'moe_w_up': nrm(ks[27], (L, N_EXPERTS, D_MODEL, 2 * D_FF), D_MODEL ** -0.5),
        'moe_b_up': nrm(ks[28], (L, N_EXPERTS, 2 * D_FF), 0.01),
        'moe_w_down': nrm(ks[29], (L, N_EXPERTS, D_FF, D_MODEL), D_FF ** -0.5),
        'moe_b_down': nrm(ks[30], (L, N_EXPERTS, D_MODEL), 0.01),
        'final_g': 1.0 + nrm(ks[31], (D_MODEL,), 0.1),
    }


def reference(x, c, ctx, c_ctx, w_mod, b_mod, norm1_g, norm2_g, w_in,
              s5_a_re, s5_a_im, s5_log_dt, s5_b_re, s5_b_im, s5_c_re, s5_c_im, s5_d, s5_w_glu,
              sgu_norm_g, sgu_w, sgu_b, w_branch, w_gate, b_gate, w_out,
              router_w, router_b, moe_w_up, moe_b_up, moe_w_down, moe_b_down, final_g):
    bsz, seq_len, _ = x.shape
    rows = seq_len // GRID_W
    n_chunks_lat = rows // CHUNK_ROWS
    ctx_len = ctx.shape[1]
    n_chunks_ctx = ctx_len // CHUNK
    xc = ctx
    for i in range(DEPTH):
        last = i == DEPTH - 1
        sh1, sc1, g1, sh2, sc2, g2 = [m[:, None, :] for m in adaln(c, w_mod[i], b_mod[i])]
        csh1, csc1, cg1, csh2, csc2, cg2 = adaln(c_ctx, w_mod[i], b_mod[i])

        h = modulate(rmsnorm(x, norm1_g[i]), sh1, sc1)
        hc = modulate(rmsnorm(xc, norm1_g[i]), csh1, csc1)
        a_bar, b_bar = s5_discretize(s5_a_re[i], s5_a_im[i], s5_log_dt[i], s5_b_re[i], s5_b_im[i])
        zc = hc @ (w_in[i][:, :S5_WIDTH] if last else w_in[i])
        uc = zc[..., :S5_WIDTH].reshape(bsz, ctx_len, S5_GROUPS, S5_GROUP)
        h0 = jnp.zeros((N_DIR, bsz, S5_GROUPS, S5_STATE), jnp.complex64)
        states_c = s5_scan(uc, a_bar, b_bar, h0)
        z = h @ w_in[i]
        u = z[..., :S5_WIDTH].reshape(bsz, seq_len, S5_GROUPS, S5_GROUP)
        states = s5_scan(u, a_bar, b_bar, states_c[:, :, -1])
        feats = branch_features(z, states, s5_c_re[i], s5_c_im[i], s5_d[i], s5_w_glu[i],
                                sgu_norm_g[i], sgu_w[i], sgu_b[i], n_chunks_lat)
        x = x + g1 * gated_merge(h, feats, w_branch[i], w_gate[i], b_gate[i], w_out[i])
        if not last:
            feats_c = branch_features(zc, states_c, s5_c_re[i], s5_c_im[i], s5_d[i], s5_w_glu[i],
                                      sgu_norm_g[i], sgu_w[i], sgu_b[i], n_chunks_ctx)
            xc = xc + cg1 * gated_merge(hc, feats_c, w_branch[i], w_gate[i], b_gate[i], w_out[i])

        h2 = modulate(rmsnorm(x, norm2_g[i]), sh2, sc2).reshape(bsz * seq_len, D_MODEL)
        if not last:
            h2c = modulate(rmsnorm(xc, norm2_g[i]), csh2, csc2).reshape(bsz * ctx_len, D_MODEL)
            y = moe_ffn(jnp.concatenate([h2, h2c], axis=0), router_w[i], router_b[i],
                        moe_w_up[i], moe_b_up[i], moe_w_down[i], moe_b_down[i])
            x = x + g2 * y[:bsz * seq_len].reshape(bsz, seq_len, D_MODEL)
            xc = xc + cg2 * y[bsz * seq_len:].reshape(bsz, ctx_len, D_MODEL)
        else:
            y = moe_ffn(h2, router_w[i], router_b[i],
                        moe_w_up[i], moe_b_up[i], moe_w_down[i], moe_b_down[i])
            x = x + g2 * y.reshape(bsz, seq_len, D_MODEL)
    return rmsnorm(x, final_g)
```

```python
import contextlib
import numpy as np
import concourse.bass as bass
import concourse.mybir as mybir

F32 = mybir.dt.float32
BF16 = mybir.dt.bfloat16
I32 = mybir.dt.int32
U32 = mybir.dt.uint32
ALU = mybir.AluOpType
AF = mybir.ActivationFunctionType
AX = mybir.AxisListType

ENGS = ("pe", "act", "dve", "pool", "sp")


class _Op:
    __slots__ = ("eng", "fn", "deps", "dma", "key", "needs_inc", "count", "idx", "inc", "kind", "flag")

    def __init__(self, eng, fn, dma, key):
        self.eng = eng
        self.fn = fn
        self.deps = []
        self.dma = dma
        self.key = key
        self.needs_inc = False
        self.count = None
        self.inc = 16
        self.kind = None
        self.flag = None


class Prog:
    def __init__(self, nc):
        self.nc = nc
        self.streams = {e: [] for e in ENGS}
        self.res = {}
        self.stack = contextlib.ExitStack()
        self.dma_keys = {}
        self.nops = 0

    def sbuf(self, name, shape, dtype=F32):
        return self.stack.enter_context(self.nc.sbuf_tensor("sb_" + name, list(shape), dtype))

    def psum(self, name, shape, dtype=F32):
        return self.stack.enter_context(self.nc.psum_tensor("pp_" + name, list(shape), dtype))

    def op(self, eng, fn, reads=(), writes=(), dma=False, key=None):
        o = _Op(eng, fn, dma, key)
        o.idx = self.nops
        self.nops += 1
        deps = set()
        for k in reads:
            r = self.res.get(k)
            if r is None:
                r = self.res[k] = [None, []]
            if r[0] is not None:
                deps.add(r[0])
        for k in writes:
            r = self.res.get(k)
            if r is None:
                r = self.res[k] = [None, []]
            if r[0] is not None:
                deps.add(r[0])
            for rd in r[1]:
                deps.add(rd)
        for k in reads:
            self.res[k][1].append(o)
        for k in writes:
            self.res[k][0] = o
            self.res[k][1] = []
        for d in deps:
            if d is o:
                continue
            if (not d.dma) and (not dma) and d.eng == "pe" and eng == "pe":
                continue
            d.needs_inc = True
            o.deps.append(d)
        if dma:
            o.needs_inc = True
            assert key is not None
        self.streams[eng].append(o)
        return o

    def dma(self, q, out, in_, reads=(), writes=(), key=None, **kw):
        if key is None:
            key = writes[0] if writes else reads[0]
        eng_attr = {"sp": "sync", "act": "scalar", "pool": "gpsimd"}[q]

        def fn(e):
            return getattr(self.nc, eng_attr).dma_start(out=out, in_=in_, **kw)

        return self.op(q, fn, reads, writes, dma=True, key=("dma", key))

    def cc(self, kind, alu, in_ap, out_ap, reads, writes, key):
        nc = self.nc

        def fn(e):
            return nc.gpsimd.collective_compute(kind, alu, replica_groups=[list(range(8))], ins=[in_ap.opt()], outs=[out_ap.opt()])

        o = self.op("pool", fn, reads, writes, dma=True, key=("cc", key))
        o.inc = 1
        return o

    def region_begin(self, flag_ap, reads):
        for eng in ENGS:
            o = self.op(eng, None, reads, [])
            o.kind = "rb"
            o.flag = flag_ap

    def region_end(self):
        for eng in ENGS:
            o = self.op(eng, None, [], [])
            o.kind = "re"

    def finish_wait_all(self, eng="sp"):
        self._final_eng = eng

    def emit(self):
        nc = self.nc
        st = self.stack
        esem = {}
        for e in ("pe", "act", "dve", "pool"):
            esem[e] = st.enter_context(nc.semaphore("s_" + e))
        dsem = {}
        dcount = {}
        for e in ENGS:
            c = 0
            for o in self.streams[e]:
                if o.dma:
                    if o.key not in dsem:
                        dsem[o.key] = st.enter_context(nc.semaphore("d%d" % len(dsem)))
                        dcount[o.key] = 0
                    dcount[o.key] += o.inc
                    o.count = dcount[o.key]
                elif o.needs_inc:
                    c += 1
                    o.count = c
        self.n_sems = len(dsem) + 4

        def semof(o):
            return dsem[o.key] if o.dma else esem[o.eng]

        final_eng = getattr(self, "_final_eng", "sp")

        def run_stream(ename, e):
            known = {}
            region = None
            for o in self.streams[ename]:
                waits = {}
                for d in o.deps:
                    s = semof(d)
                    k = id(s)
                    if d.count > known.get(k, 0):
                        if k not in waits or waits[k][1] < d.count:
                            waits[k] = (s, d.count)
                for k, (s, v) in waits.items():
                    e.wait_ge(s, v)
                    known[k] = v
                if o.kind == "rb":
                    val = e.value_load(o.flag, min_val=0, max_val=1)
                    guard = e.If(val)
                    guard.__enter__()
                    region = [guard, dict(known), {}]
                    continue
                if o.kind == "re":
                    region[0].__exit__(None, None, None)
                    if region[2]:
                        with e.Else():
                            for s_, n_ in region[2].values():
                                e.sem_inc(s_, n_)
                    known = region[1]
                    region = None
                    continue
                ins = o.fn(e)
                if o.needs_inc:
                    if o.dma:
                        ins.then_inc(dsem[o.key], o.inc)
                        s_, n_ = dsem[o.key], o.inc
                    else:
                        ins.then_inc(esem[o.eng], 1)
                        s_, n_ = esem[o.eng], 1
                    if region is not None:
                        prev = region[2].get(id(s_), (s_, 0))
                        region[2][id(s_)] = (s_, prev[1] + n_)
            if ename == final_eng:
                for key, s in dsem.items():
                    if dcount[key] > known.get(id(s), 0):
                        e.wait_ge(s, dcount[key])

        with nc.Block() as block:
            @block.sync
            def _(e):
                run_stream("sp", e)

            @block.scalar
            def _(e):
                run_stream("act", e)

            @block.vector
            def _(e):
                run_stream("dve", e)

            @block.gpsimd
            def _(e):
                run_stream("pool", e)

            @block.tensor
            def _(e):
                run_stream("pe", e)
        st.close()
import numpy as np
import ml_dtypes
from concourse.bass_utils import run_bass_kernel_spmd

NCORES = 8
D = 2048
SEQ = 8192
CTX = 256
NTOK = SEQ + CTX
TLAT = SEQ // NCORES
TL = TLAT + CTX
NT = TL // 128
KC = D // 128
EPS = 1e-6
TBLK = [(0, 512), (512, 512), (1024, 256)]


def mm(p, out, lhsT, rhs, start, stop, reads, writes):
    p.op("pe", lambda e: p.nc.tensor.matmul(out, lhsT=lhsT, rhs=rhs, start=start, stop=stop), reads, writes)


def tr(p, out, in_, ident, reads, writes):
    p.op("pe", lambda e: p.nc.tensor.transpose(out, in_, ident), list(reads) + ["ident"], writes)


def act(p, out, in_, func, reads, writes, scale=1.0, bias=0.0, accum_out=None):
    def fn(e):
        kw = {}
        if accum_out is not None:
            kw["accum_out"] = accum_out
        return p.nc.scalar.activation(out=out, in_=in_, func=func, scale=scale, bias=bias, **kw)
    p.op("act", fn, reads, writes)


def _veng(p, eng):
    return p.nc.vector if eng == "dve" else p.nc.gpsimd


def tt(p, eng, out, in0, in1, op, reads, writes):
    eng = "dve"
    p.op(eng, lambda e: _veng(p, eng).tensor_tensor(out=out, in0=in0, in1=in1, op=op), reads, writes)


def ts(p, eng, out, in0, s1, op0, reads, writes, s2=None, op1=None):
    eng = "dve"

    def fn(e):
        if op1 is None:
            return _veng(p, eng).tensor_scalar(out=out, in0=in0, scalar1=s1, scalar2=None, op0=op0)
        return _veng(p, eng).tensor_scalar(out=out, in0=in0, scalar1=s1, scalar2=s2, op0=op0, op1=op1)
    p.op(eng, fn, reads, writes)


def stt(p, eng, out, in0, scalar, in1, op0, op1, reads, writes):
    assert eng == "dve"
    p.op(eng, lambda e: _veng(p, eng).scalar_tensor_tensor(out=out, in0=in0, scalar=scalar, in1=in1, op0=op0, op1=op1), reads, writes)


def cp(p, eng, out, in_, reads, writes):
    if eng == "act":
        p.op("act", lambda e: p.nc.scalar.copy(out=out, in_=in_), reads, writes)
    else:
        eng = "dve"
        p.op(eng, lambda e: _veng(p, eng).tensor_copy(out=out, in_=in_), reads, writes)


def recip(p, out, in_, reads, writes):
    p.op("dve", lambda e: p.nc.vector.reciprocal(out=out, in_=in_), reads, writes)


def memset(p, eng, ap, val, writes):
    eng = "dve"
    p.op(eng, lambda e: _veng(p, eng).memset(ap, val), [], writes)


def make_ident(p):
    nc = p.nc
    io = p.sbuf("io_id", [128, 128])
    pc = p.sbuf("pc_id", [128, 1])
    ident = p.sbuf("ident", [128, 128])
    p.op("pool", lambda e: nc.gpsimd.iota(io[:], pattern=[[1, 128]], base=0, channel_multiplier=0, allow_small_or_imprecise_dtypes=True), [], ["io_id"])
    p.op("pool", lambda e: nc.gpsimd.iota(pc[:], pattern=[[0, 1]], base=0, channel_multiplier=1, allow_small_or_imprecise_dtypes=True), [], ["pc_id"])
    ts(p, "dve", ident[:], io[:], pc[:, 0:1], ALU.is_equal, ["io_id", "pc_id"], ["ident"])
    return ident, io, pc


def dram_in(nc, name, shape, dt=F32):
    return nc.dram_tensor(name, list(shape), dt, kind="ExternalInput").ap()


def dram_out(nc, name, shape, dt=F32):
    return nc.dram_tensor(name, list(shape), dt, kind="ExternalOutput").ap()


def adaln_T(p, P, c2T, wmod, bmodT, nchunk, wts=None, wkey="wmt"):
    nc = p.nc
    c2s = p.sbuf("c2s", [128, 32])
    bm = p.sbuf("bm", [128, nchunk])
    modT = p.sbuf("modT", [128, nchunk, 2])
    p.dma("pool", c2s[:], c2T, writes=["c2s"])
    p.dma("pool", bm[:], bmodT, writes=["bm"])
    act(p, c2s[:], c2s[:], AF.Silu, ["c2s"], ["c2s"])
    if wts is None:
        wts = [p.sbuf("wmt%d" % i, [128, KC, 256]) for i in range(2)]
    modps = P[7]
    ntile = nchunk // 2
    for t in range(ntile):
        s = t % 2
        p.dma("sp", wts[s][:], wmod[:, t * 256:(t + 1) * 256].rearrange("(k p) c -> p k c", p=128), writes=[(wkey, s)])
        for j in range(2):
            cc = t * 2 + j
            for k in range(KC):
                mm(p, modps[:, cc * 2:cc * 2 + 2], wts[s][:, k, j * 128:(j + 1) * 128], c2s[:, 2 * k:2 * k + 2],
                   k == 0, k == KC - 1, [(wkey, s), "c2s"], [("ps", 7)])
    for r in range(2):
        tt(p, "dve", modT[:, :, r], modps[:, r:2 * nchunk:2], bm[:], ALU.add, [("ps", 7), "bm"], ["modT"])
    return modT


def build_pre(nc):
    p = Prog(nc)
    x = dram_in(nc, "x", [TL, D])
    c2T = dram_in(nc, "c2T", [128, 32])
    wmod = dram_in(nc, "wmod", [D, 4096])
    bmodT = dram_in(nc, "bmodT", [128, 32])
    g1nT = dram_in(nc, "g1nT", [128, KC])
    w_in = dram_in(nc, "w_in", [D, D])
    gv = dram_in(nc, "gv", [1, 512])
    sguw = dram_in(nc, "sguw", [4, 128, 128])
    sgub = dram_in(nc, "sgub", [1, 512])
    hT_o = dram_out(nc, "hT", [128, KC, TL], BF16)
    zT_o = dram_out(nc, "zT", [1024, TL])
    ysgu_o = dram_out(nc, "ysgu", [512, TL])

    P = [p.psum("ps%d" % i, [128, 512]) for i in range(8)]
    ident, io, pc = make_ident(p)

    modT = adaln_T(p, P, c2T, wmod, bmodT, 32)
    g1n = p.sbuf("g1n", [128, KC])
    p.dma("pool", g1n[:], g1nT, writes=["g1n"])
    A1 = p.sbuf("A1", [128, 2, KC])
    for r in range(2):
        stt(p, "dve", A1[:, r, :], modT[:, 16:32, r], 1.0, g1n[:], ALU.add, ALU.mult, ["modT", "g1n"], ["A1"])

    wsq = p.sbuf("wsq", [128, 4, 128])
    WsT = p.sbuf("WsT", [128, 4, 128])
    bsb = p.sbuf("bsb", [128, 512])
    gvb = p.sbuf("gvb", [128, 512])
    p.dma("pool", wsq[:], sguw.rearrange("h q k -> q h k"), writes=["wsq"])
    p.dma("pool", bsb[:], sgub.partition_broadcast(128), writes=["bsb"])
    p.dma("pool", gvb[:], gv.partition_broadcast(128), writes=["gvb"])
    for hh in range(4):
        tr(p, P[6][:, hh * 128:(hh + 1) * 128], wsq[:, hh, :], ident[:], ["wsq"], [("ps", 6)])
    cp(p, "dve", WsT[:].rearrange("p h q -> p (h q)"), P[6][:], [("ps", 6)], ["WsT"])

    hT = p.sbuf("hTs", [128, KC, TL], BF16)
    xb = [p.sbuf("xb%d" % i, [128, D]) for i in range(2)]
    junk = p.sbuf("junk", [128, D])
    ssq = p.sbuf("ssq", [128, NT])
    rs = p.sbuf("rs", [128, NT])
    ev = 0
    for t in range(NT):
        r = 0 if t < 8 else 1
        s = t % 2
        xt = xb[s]
        p.dma("sp", xt[:], x[t * 128:(t + 1) * 128, :], writes=[("xb", s)])
        act(p, junk[:], xt[:], AF.Square, [("xb", s)], ["junk", ("ssq", t)], accum_out=ssq[:, t:t + 1])
        act(p, rs[:, t:t + 1], ssq[:, t:t + 1], AF.Sqrt, [("ssq", t)], [("rs", t)], scale=1.0 / D, bias=EPS)
        recip(p, rs[:, t:t + 1], rs[:, t:t + 1], [("rs", t)], [("rs", t)])
        ts(p, "pool", xt[:], xt[:], rs[:, t:t + 1], ALU.mult, [("xb", s), ("rs", t)], [("xb", s)])
        for kq in range(4):
            b = (t * 4 + kq) % 4
            for j in range(4):
                k = kq * 4 + j
                tr(p, P[b][:, j * 128:(j + 1) * 128], xt[:, k * 128:(k + 1) * 128], ident[:], [("xb", s)], [("ps", b)])
            for j in range(4):
                k = kq * 4 + j
                o = hT[:, k, t * 128:(t + 1) * 128]
                i_ = P[b][:, j * 128:(j + 1) * 128]
                if ev % 2 == 0:
                    ts(p, "dve", o, i_, A1[:, r, k:k + 1], ALU.mult, [("ps", b), "A1", "modT"], [("hT", t)],
                       s2=modT[:, k, r:r + 1], op1=ALU.add)
                else:
                    act(p, o, i_, AF.Identity, [("ps", b), "A1", "modT"], [("hT", t)],
                        scale=A1[:, r, k:k + 1], bias=modT[:, k, r:r + 1])
                ev += 1
    for k in range(0, KC, 4):
        p.dma("pool", hT_o[:, k:k + 4, :], hT[:, k:k + 4, :], reads=[("hT", t) for t in range(NT)], key=("hTo", k))

    wst = [p.sbuf("wst%d" % i, [128, KC, 256]) for i in range(2)]
    winbf = p.sbuf("winbf", [128, KC, 512], BF16)
    usgu = p.sbuf("usgu", [128, 4, TL])
    zst = [p.sbuf("zst%d" % i, [128, TL]) for i in range(2)]
    vt = p.sbuf("vt", [128, 512])
    vn = p.sbuf("vn", [128, 512])
    ssv = p.sbuf("ssv", [128, NT])
    rsv = p.sbuf("rsv", [128, NT])
    tmp = p.sbuf("tmp", [128, 128])
    allh = [("hT", t) for t in range(NT)]
    nz = 0
    for cb in range(4):
        for hf in range(2):
            s = hf
            p.dma("sp", wst[s][:], w_in[:, cb * 512 + hf * 256: cb * 512 + (hf + 1) * 256].rearrange("(k p) c -> p k c", p=128),
                  writes=[("wst", s)])
            cp(p, "pool" if hf else "dve", winbf[:, :, hf * 256:(hf + 1) * 256], wst[s][:], [("wst", s)], ["winbf"])
        if cb < 3:
            for c4 in range(4):
                zs = zst[nz % 2]
                for bi, (t0, tn) in enumerate(TBLK):
                    b = (nz * 3 + bi) % 4
                    for k in range(KC):
                        mm(p, P[b][:, 0:tn], winbf[:, k, c4 * 128:(c4 + 1) * 128], hT[:, k, t0:t0 + tn], k == 0, k == KC - 1,
                           ["winbf"] + allh, [("ps", b)])
                    if cb < 2:
                        cp(p, "act" if bi % 2 else "dve", zs[:, t0:t0 + tn], P[b][:, 0:tn], [("ps", b)], [("zst", nz % 2)])
                    else:
                        act(p, usgu[:, c4, t0:t0 + tn], P[b][:, 0:tn], AF.Gelu_apprx_tanh, [("ps", b)],
                            [("usgu", c4, t) for t in range(t0 // 128, (t0 + tn) // 128)])
                if cb < 2:
                    row = (cb * 4 + c4) * 128
                    p.dma("pool", zT_o[row:row + 128, :], zs[:], reads=[("zst", nz % 2)], key=("zo", nz % 2))
                nz += 1
        else:
            for t in range(NT):
                b = 4 + t % 2
                for k in range(KC):
                    mm(p, P[b][:], hT[:, k, t * 128:(t + 1) * 128], winbf[:, k, :], k == 0, k == KC - 1, ["winbf", ("hT", t)], [("ps", b)])
                act(p, vt[:], P[b][:], AF.Gelu_apprx_tanh, [("ps", b)], ["vt"])
                act(p, junk[:, 0:512], vt[:], AF.Square, ["vt"], ["junk", ("ssv", t)], accum_out=ssv[:, t:t + 1])
                act(p, rsv[:, t:t + 1], ssv[:, t:t + 1], AF.Sqrt, [("ssv", t)], [("rsv", t)], scale=1.0 / 512, bias=EPS)
                recip(p, rsv[:, t:t + 1], rsv[:, t:t + 1], [("rsv", t)], [("rsv", t)])
                stt(p, "dve", vn[:], vt[:], rsv[:, t:t + 1], gvb[:], ALU.mult, ALU.mult, ["vt", ("rsv", t), "gvb"], ["vn"])
                for hh in range(4):
                    mm(p, P[6][:, hh * 128:(hh + 1) * 128], vn[:, hh * 128:(hh + 1) * 128], WsT[:, hh, :], True, True, ["vn", "WsT"], [("ps", 6)])
                for hh in range(4):
                    tt(p, "dve", tmp[:], P[6][:, hh * 128:(hh + 1) * 128], bsb[:, hh * 128:(hh + 1) * 128], ALU.add, [("ps", 6), "bsb"], ["tmp"])
                    u_ = usgu[:, hh, t * 128:(t + 1) * 128]
                    tt(p, "pool", u_, u_, tmp[:], ALU.mult, ["tmp", ("usgu", hh, t)], [("usgu", hh, t)])
    for hh in range(4):
        p.dma("pool", ysgu_o[hh * 128:(hh + 1) * 128, :], usgu[:, hh, :], reads=[("usgu", hh, t) for t in range(NT)], key=("yso", hh))
    p.emit()
    return p


NBLK = [(i * 512, min(512, NTOK - i * 512)) for i in range((NTOK + 511) // 512)]
TWO_PI = 6.283185307179586


def frac_sincos(p, eng, x, xi, sin_out, cos_out, key, n_reads):
    cp(p, eng, xi, x, n_reads + [key + "x"], [key + "xi"])
    tt(p, eng, x, x, xi, ALU.subtract, [key + "x", key + "xi"], [key + "x"])
    if sin_out is not None:
        act(p, sin_out, x, AF.Sin, [key + "x"], [key + "sin"], scale=TWO_PI)
    if cos_out is not None:
        act(p, x, x, AF.Abs, [key + "x", key + "sin"], [key + "x"])
        act(p, cos_out, x, AF.Sin, [key + "x"], [key + "cos"], scale=-TWO_PI, bias=p.halfpi[:, 0:1])


def build_mix(nc):
    p = Prog(nc)
    uf = dram_in(nc, "uf", [64, NTOK])
    ub = dram_in(nc, "ub", [64, NTOK])
    spar = dram_in(nc, "spar", [128, 4, 3])
    bre_d = dram_in(nc, "bre", [128, 4, 32])
    bim_d = dram_in(nc, "bim", [128, 4, 32])
    cre_d = dram_in(nc, "cre", [128, 4, 64])
    cim_d = dram_in(nc, "cim", [128, 4, 64])
    zfl = dram_in(nc, "zfl", [128, SEQ])
    zfc = dram_in(nc, "zfc", [128, CTX])
    kbv_d = dram_in(nc, "kbv", [128, 8])
    ysf_o = dram_out(nc, "ysf", [64, NTOK])
    ysb_o = dram_out(nc, "ysb", [64, NTOK])
    yf_o = dram_out(nc, "yf", [128, 4096])
    yfc_o = dram_out(nc, "yfc", [128, CTX])

    P = [p.psum("ps%d" % i, [128, 512]) for i in range(8)]
    ident, io, pc = make_ident(p)
    p.halfpi = p.sbuf("halfpi", [128, 1])
    memset(p, "dve", p.halfpi[:], TWO_PI / 4, ["halfpi"])
    jio = p.sbuf("jio", [128, 512])
    p.op("pool", lambda e: nc.gpsimd.iota(jio[:], pattern=[[1, 512]], base=0, channel_multiplier=0, allow_small_or_imprecise_dtypes=True), [], ["jio"])

    sp_ = p.sbuf("spar", [128, 4, 3])
    p.dma("pool", sp_[:], spar, writes=["spar"])
    bre = p.sbuf("bre", [128, 4, 32]); bim = p.sbuf("bim", [128, 4, 32])
    cre = p.sbuf("cre", [128, 4, 64]); ncim = p.sbuf("ncim", [128, 4, 64])
    p.dma("pool", bre[:], bre_d, writes=["bre"]); p.dma("pool", bim[:], bim_d, writes=["bim"])
    p.dma("pool", cre[:], cre_d, writes=["cre"]); p.dma("pool", ncim[:], cim_d, writes=["ncim"])
    ts(p, "dve", ncim[:], ncim[:], -1.0, ALU.mult, ["ncim"], ["ncim"])
    sm = p.sbuf("sm", [128, 16, 4])
    SM = {n: sm[:, i, :] for i, n in enumerate(["dt", "ar", "th", "r", "x", "cs", "sn", "abr", "abi", "den", "cr", "ci", "t1", "t2", "nr", "x2"])}
    smi = p.sbuf("smi", [128, 4], I32)
    are, aim, ldt = sp_[:, :, 0], sp_[:, :, 1], sp_[:, :, 2]
    K_ = ["spar", "sm"]
    act(p, SM["dt"], ldt, AF.Exp, ["spar"], ["sm"])
    tt(p, "dve", SM["ar"], are, SM["dt"], ALU.mult, K_, ["sm"])
    tt(p, "dve", SM["th"], aim, SM["dt"], ALU.mult, K_, ["sm"])
    ts(p, "dve", SM["th"], SM["th"], 1.0 / TWO_PI, ALU.mult, K_, ["sm"])
    act(p, SM["r"], SM["ar"], AF.Exp, ["sm"], ["sm"])
    cp(p, "dve", SM["x"], SM["th"], K_, ["sm"])
    cp(p, "dve", smi[:], SM["x"], K_, ["smi"])
    tt(p, "dve", SM["x"], SM["x"], smi[:], ALU.subtract, ["sm", "smi"], ["sm"])
    act(p, SM["sn"], SM["x"], AF.Sin, ["sm"], ["sm"], scale=TWO_PI)
    act(p, SM["x2"], SM["x"], AF.Abs, ["sm"], ["sm"])
    act(p, SM["cs"], SM["x2"], AF.Sin, ["sm", "halfpi"], ["sm"], scale=-TWO_PI, bias=p.halfpi[:, 0:1])
    tt(p, "dve", SM["abr"], SM["r"], SM["cs"], ALU.mult, ["sm"], ["sm"])
    tt(p, "dve", SM["abi"], SM["r"], SM["sn"], ALU.mult, ["sm"], ["sm"])
    ts(p, "dve", SM["nr"], SM["abr"], -1.0, ALU.add, ["sm"], ["sm"])
    tt(p, "dve", SM["den"], are, are, ALU.mult, K_, ["sm"])
    tt(p, "dve", SM["t1"], aim, aim, ALU.mult, K_, ["sm"])
    tt(p, "dve", SM["den"], SM["den"], SM["t1"], ALU.add, ["sm"], ["sm"])
    recip(p, SM["den"], SM["den"], ["sm"], ["sm"])
    tt(p, "dve", SM["t1"], SM["nr"], are, ALU.mult, K_, ["sm"])
    tt(p, "dve", SM["t2"], SM["abi"], aim, ALU.mult, K_, ["sm"])
    tt(p, "dve", SM["cr"], SM["t1"], SM["t2"], ALU.add, ["sm"], ["sm"])
    tt(p, "dve", SM["cr"], SM["cr"], SM["den"], ALU.mult, ["sm"], ["sm"])
    tt(p, "dve", SM["t1"], SM["abi"], are, ALU.mult, K_, ["sm"])
    tt(p, "dve", SM["t2"], SM["nr"], aim, ALU.mult, K_, ["sm"])
    tt(p, "dve", SM["ci"], SM["t1"], SM["t2"], ALU.subtract, ["sm"], ["sm"])
    tt(p, "dve", SM["ci"], SM["ci"], SM["den"], ALU.mult, ["sm"], ["sm"])
    bbr = p.sbuf("bbr", [128, 4, 32]); bbi = p.sbuf("bbi", [128, 4, 32]); btmp = p.sbuf("btmp", [128, 32])
    lhsB = p.sbuf("lhsB", [32, 4, 2, 128])
    for ti in range(4):
        crc, cic = SM["cr"][:, ti:ti + 1], SM["ci"][:, ti:ti + 1]
        ts(p, "dve", btmp[:], bim[:, ti, :], cic, ALU.mult, ["bim", "sm"], ["btmp"])
        stt(p, "dve", bbr[:, ti, :], bre[:, ti, :], crc, btmp[:], ALU.mult, ALU.subtract, ["bre", "sm", "btmp"], ["bbr"])
        ts(p, "dve", btmp[:], bre[:, ti, :], cic, ALU.mult, ["bre", "sm", "bbr"], ["btmp"])
        stt(p, "dve", bbi[:, ti, :], bim[:, ti, :], crc, btmp[:], ALU.mult, ALU.add, ["bim", "sm", "btmp"], ["bbi"])
    for ti in range(4):
        bk = 6 + ti // 2
        for ri, src in enumerate((bbr, bbi)):
            c0 = ((ti % 2) * 2 + ri) * 128
            tr(p, P[bk][0:32, c0:c0 + 128], src[:, ti, :], ident[:], ["bbr", "bbi"], [("ps", bk)])
    for hlf in range(2):
        cp(p, "dve", lhsB[:, 2 * hlf:2 * hlf + 2, :, :].rearrange("p a b c -> p (a b c)"), P[6 + hlf][0:32, :], [("ps", 6 + hlf)], ["lhsB"])
    tabc = p.sbuf("tabc", [128, 4, 512]); tabs = p.sbuf("tabs", [128, 4, 512])
    tx = p.sbuf("tx", [128, 512]); txi = p.sbuf("txi", [128, 512], I32)
    for ti in range(4):
        ts(p, "dve", tx[:], jio[:], SM["th"][:, ti:ti + 1], ALU.mult, ["jio", "sm", "Tcos", "Tsin"], ["Tx"])
        frac_sincos(p, "dve", tx[:], txi[:], tabs[:, ti, :], tabc[:, ti, :], "T", [])

    NW = 2
    ubuf = [[p.sbuf("u%d_%d" % (ti, i), [32, 512]) for i in range(NW)] for ti in range(4)]
    W = {}
    for nm in ["bre", "bim", "t1", "t2", "inre", "inim", "wre", "wim", "sre", "sim"]:
        W[nm] = [p.sbuf("w_%s%d" % (nm, i), [128, 512]) for i in range(2)]
    carry = p.sbuf("carry", [128, 4, 2])
    ctmp = p.sbuf("ctmp", [128, 4, 2])
    memset(p, "dve", carry[:], 0.0, ["carry"])
    yst = [[p.sbuf("yst%d_%d" % (d, i), [64, 512]) for i in range(2)] for d in range(2)]
    usrc = [uf, ub]
    it = 0
    for bi, (t0, tn) in enumerate(NBLK):
        for d in range(2):
            for gp in range(2):
                ti = d * 2 + gp
                ws = it % 2
                us = bi % NW
                u = ubuf[ti][us]
                p.dma("sp", u[:, 0:tn], usrc[d][gp * 32:(gp + 1) * 32, t0:t0 + tn], writes=[("u", ti, us)])
                b0, b1 = (0, 1) if it % 2 == 0 else (2, 3)
                mm(p, P[b0][:, 0:tn], lhsB[:, ti, 0, :], u[:, 0:tn], True, True, ["lhsB", ("u", ti, us)], [("ps", b0)])
                mm(p, P[b1][:, 0:tn], lhsB[:, ti, 1, :], u[:, 0:tn], True, True, ["lhsB", ("u", ti, us)], [("ps", b1)])
                k = lambda nm: ("w", nm, ws)
                a = lambda nm: W[nm][ws][:, 0:tn]
                cp(p, "act", a("bre"), P[b0][:, 0:tn], [("ps", b0)], [k("bre")])
                cp(p, "act", a("bim"), P[b1][:, 0:tn], [("ps", b1)], [k("bim")])
                cs_, sn_ = tabc[:, ti, 0:tn], tabs[:, ti, 0:tn]
                tt(p, "pool", a("t1"), a("bre"), cs_, ALU.mult, [k("bre"), "Tcos"], [k("t1")])
                tt(p, "pool", a("t2"), a("bim"), sn_, ALU.mult, [k("bim"), "Tsin"], [k("t2")])
                tt(p, "pool", a("inre"), a("t1"), a("t2"), ALU.add, [k("t1"), k("t2")], [k("inre")])
                tt(p, "pool", a("t1"), a("bim"), cs_, ALU.mult, [k("bim"), "Tcos"], [k("t1")])
                tt(p, "pool", a("t2"), a("bre"), sn_, ALU.mult, [k("bre"), "Tsin"], [k("t2")])
                tt(p, "pool", a("inim"), a("t1"), a("t2"), ALU.subtract, [k("t1"), k("t2")], [k("inim")])
                rr = SM["r"][:, ti:ti + 1]
                p.op("dve", lambda e, o=a("wre"), i=a("inre"), c=carry[:, ti, 0:1], rr=rr, tn=tn: nc.vector.tensor_tensor_scan(
                    out=o, data0=rr.to_broadcast([128, tn]), data1=i, initial=c, op0=ALU.mult, op1=ALU.add),
                    [k("inre"), ("carry", ti), "sm"], [k("wre")])
                p.op("dve", lambda e, o=a("wim"), i=a("inim"), c=carry[:, ti, 1:2], rr=rr, tn=tn: nc.vector.tensor_tensor_scan(
                    out=o, data0=rr.to_broadcast([128, tn]), data1=i, initial=c, op0=ALU.mult, op1=ALU.add),
                    [k("inim"), ("carry", ti), "sm"], [k("wim")])
                tt(p, "dve", a("t1"), a("wre"), cs_, ALU.mult, [k("wre"), "Tcos", k("inre"), k("inim")], [k("t1")])
                tt(p, "dve", a("t2"), a("wim"), sn_, ALU.mult, [k("wim"), "Tsin", k("inre"), k("inim")], [k("t2")])
                tt(p, "dve", a("sre"), a("t1"), a("t2"), ALU.subtract, [k("t1"), k("t2")], [k("sre")])
                tt(p, "pool", a("inre"), a("wre"), sn_, ALU.mult, [k("wre"), "Tsin"], [k("inre")])
                tt(p, "pool", a("inim"), a("wim"), cs_, ALU.mult, [k("wim"), "Tcos"], [k("inim")])
                tt(p, "pool", a("sim"), a("inre"), a("inim"), ALU.add, [k("inre"), k("inim")], [k("sim")])
                sl_re, sl_im = W["sre"][ws][:, tn - 1:tn], W["sim"][ws][:, tn - 1:tn]
                csc, snc = SM["cs"][:, ti:ti + 1], SM["sn"][:, ti:ti + 1]
                ts(p, "dve", ctmp[:, ti, 0:1], sl_im, snc, ALU.mult, [k("sim"), "sm"], [("ctmp", ti)])
                stt(p, "dve", carry[:, ti, 0:1], sl_re, csc, ctmp[:, ti, 0:1], ALU.mult, ALU.subtract, [k("sre"), ("ctmp", ti), "sm"], [("carry", ti)])
                ts(p, "dve", ctmp[:, ti, 1:2], sl_re, snc, ALU.mult, [k("sre"), "sm"], [("ctmp", ti)])
                stt(p, "dve", carry[:, ti, 1:2], sl_im, csc, ctmp[:, ti, 1:2], ALU.mult, ALU.add, [k("sim"), ("ctmp", ti), "sm"], [("carry", ti)])
                yb = 4 + d
                mm(p, P[yb][0:64, 0:tn], cre[:, ti, :], a("sre"), gp == 0, False, ["cre", k("sre")], [("ps", yb)])
                mm(p, P[yb][0:64, 0:tn], ncim[:, ti, :], a("sim"), False, gp == 1, ["ncim", k("sim")], [("ps", yb)])
                it += 1
            ys = yst[d][bi % 2]
            cp(p, "act", ys[:, 0:tn], P[4 + d][0:64, 0:tn], [("ps", 4 + d)], [("yst", d, bi % 2)])
            p.dma("pool", (ysf_o if d == 0 else ysb_o)[:, t0:t0 + tn], ys[:, 0:tn], reads=[("yst", d, bi % 2)], key=("yso", d, bi % 2))

    kbv = p.sbuf("kbv", [128, 8])
    p.dma("pool", kbv[:], kbv_d, writes=["kbv"])
    cs128 = p.sbuf("cs128", [128, 256])
    fx = p.sbuf("fx", [128, 512]); fxi = p.sbuf("fxi", [128, 512], I32)
    ts(p, "dve", fx[:, 0:128], io[:], pc[:, 0:1], ALU.mult, ["io_id", "pc_id"], ["Fx"], s2=1.0 / 128, op1=ALU.mult)
    frac_sincos(p, "dve", fx[:, 0:128], fxi[:, 0:128], cs128[:, 128:256], cs128[:, 0:128], "F", [])
    nval = p.sbuf("nval", [128, 64])
    p.op("pool", lambda e: nc.gpsimd.iota(nval[:], pattern=[[128, 64]], base=0, channel_multiplier=1, allow_small_or_imprecise_dtypes=True), [], ["nval"])
    bx = p.sbuf("bx", [128, 64, 8]); bxi = p.sbuf("bxi", [128, 512], I32)
    cbt = p.sbuf("cbt", [128, 64, 8]); sbt = p.sbuf("sbt", [128, 64, 8]); ncbt = p.sbuf("ncbt", [128, 64, 8])
    for kb in range(8):
        ts(p, "dve", bx[:, :, kb], nval[:], kbv[:, kb:kb + 1], ALU.mult, ["nval", "kbv"], ["Bx"], s2=1.0 / 16, op1=ALU.mult)
    bxf = bx[:].rearrange("p a b -> p (a b)")
    frac_sincos(p, "dve", bxf, bxi[:], sbt[:].rearrange("p a b -> p (a b)"), cbt[:].rearrange("p a b -> p (a b)"), "B", [])
    ts(p, "dve", ncbt[:].rearrange("p a b -> p (a b)"), cbt[:].rearrange("p a b -> p (a b)"), -1.0, ALU.mult, ["Bcos"], ["ncbt"])

    def fnet(zsrc, N, ntile, nkb, kw, out_ap, scale, tagname):
        CH = min(N, 2048)
        zsb = [p.sbuf("zs%d_%s" % (i, tagname), [128, CH]) for i in range(2 if N > CH else 1)]
        AB = p.sbuf("AB_" + tagname, [128, ntile, 256], BF16 if ntile > 2 else F32)
        for nt in range(ntile):
            b = nt % 2
            ci, off = divmod(nt * 128, CH)
            zs = zsb[ci % 2]
            if off == 0:
                p.dma("sp", zs[:], zsrc[:, ci * CH:(ci + 1) * CH], writes=[("zs" + tagname, ci % 2)])
            mm(p, P[b][:, 0:256], zs[:, off:off + 128], cs128[:], True, True, [("zs" + tagname, ci % 2), "Fcos", "Fsin"], [("ps", b)])
            cp(p, "act" if nt % 2 else "dve", AB[:, nt, :], P[b][:, 0:256], [("ps", b)], [("AB" + tagname, nt)])
        Ca = [p.sbuf("Ca%d_%s" % (i, tagname), [128, kw], BF16) for i in range(2)]
        Sa = [p.sbuf("Sa%d_%s" % (i, tagname), [128, kw], BF16) for i in range(2)]
        PQ = [p.sbuf("PQ%d_%s" % (i, tagname), [128, 2, 128], BF16) for i in range(4)]
        qt = [p.sbuf("qt%d_%s" % (i, tagname), [128, 2, 128]) for i in range(2)]
        cx = p.sbuf("cx_" + tagname, [128, kw]); cxi = p.sbuf("cxi_" + tagname, [128, kw], I32)
        npq = 0
        for nt in range(ntile):
            s = nt % 2
            tk = "C" + tagname
            ts(p, "dve", cx[:], jio[:, 0:kw], nval[:, nt:nt + 1], ALU.mult, ["jio", "nval", tk + "sin", tk + "cos"], [tk + "x"], s2=1.0 / N, op1=ALU.mult)
            cp(p, "dve", cxi[:], cx[:], [tk + "x"], [tk + "xi"])
            tt(p, "dve", cx[:], cx[:], cxi[:], ALU.subtract, [tk + "x", tk + "xi"], [tk + "x"])
            act(p, Sa[s][:], cx[:], AF.Sin, [tk + "x"], [tk + "sin", ("Sa" + tagname, s)], scale=TWO_PI)
            act(p, cx[:], cx[:], AF.Abs, [tk + "x", tk + "sin"], [tk + "x"])
            act(p, Ca[s][:], cx[:], AF.Sin, [tk + "x", "halfpi"], [tk + "cos", ("Ca" + tagname, s)], scale=-TWO_PI, bias=p.halfpi[:, 0:1])
            A_, B_ = AB[:, nt, 0:128], AB[:, nt, 128:256]
            for kb in range(nkb):
                pq = PQ[npq % 4]; q = qt[npq % 2]; eng = "dve" if npq % 2 == 0 else "pool"
                kq = ("PQ" + tagname, npq % 4); kt = ("qt" + tagname, npq % 2)
                if nkb == 1:
                    cp(p, eng, pq[:, 0, :], A_, [("AB" + tagname, nt)], [kq])
                    ts(p, eng, pq[:, 1, :], B_, -1.0, ALU.mult, [("AB" + tagname, nt)], [kq])
                else:
                    cb_, sb_, ncb_ = cbt[:, nt, kb:kb + 1], sbt[:, nt, kb:kb + 1], ncbt[:, nt, kb:kb + 1]
                    ts(p, "pool", q[:, 0, :], B_, sb_, ALU.mult, [("AB" + tagname, nt), "Bsin"], [kt])
                    ts(p, "pool", q[:, 1, :], A_, sb_, ALU.mult, [("AB" + tagname, nt), "Bsin"], [kt])
                    stt(p, "dve", pq[:, 0, :], A_, cb_, q[:, 0, :], ALU.mult, ALU.subtract, [("AB" + tagname, nt), "Bcos", kt], [kq])
                    stt(p, "dve", pq[:, 1, :], B_, ncb_, q[:, 1, :], ALU.mult, ALU.subtract, [("AB" + tagname, nt), "ncbt", kt], [kq])
                mm(p, P[kb][:, 0:kw], pq[:, 0, :], Ca[s][:], nt == 0, False, [kq, ("Ca" + tagname, s)], [("ps", kb)])
                mm(p, P[kb][:, 0:kw], pq[:, 1, :], Sa[s][:], False, nt == ntile - 1, [kq, ("Sa" + tagname, s)], [("ps", kb)])
                npq += 1
        yst_ = p.sbuf("yfst_" + tagname, [128, nkb * kw])
        for kb in range(nkb):
            act(p, yst_[:, kb * kw:(kb + 1) * kw], P[kb][:, 0:kw], AF.Copy, [("ps", kb)], ["yfst" + tagname], scale=scale)
        p.dma("pool", out_ap, yst_[:], reads=["yfst" + tagname], key="yfo" + tagname)

    fnet(zfc, CTX, 2, 1, 256, yfc_o, 1.0 / float(np.sqrt(CTX * 128.0)), "c")
    fnet(zfl, SEQ, 64, 8, 512, yf_o, 1.0 / 1024.0, "l")
    p.emit()
    return p


def build_posta1(nc):
    p = Prog(nc)
    hT_d = dram_in(nc, "hT", [128, KC, TL], BF16)
    uT_d = dram_in(nc, "uT", [512, TL])
    ysf_d = dram_in(nc, "ysf", [512, TL])
    ysb_d = dram_in(nc, "ysb", [512, TL])
    yf_d = dram_in(nc, "yf", [512, TL])
    ysgu_d = dram_in(nc, "ysgu", [512, TL])
    s5d_d = dram_in(nc, "s5d", [128, 4])
    wglu_d = dram_in(nc, "wglu", [512, 512])
    wbr_d = dram_in(nc, "wbr", [3, 512, D])
    wgate_d = dram_in(nc, "wgate", [D, 3 * D])
    bgT_d = dram_in(nc, "bgT", [128, 48])
    mT_o = dram_out(nc, "mT", [128, KC, TL], BF16)

    P = [p.psum("ps%d" % i, [128, 512]) for i in range(8)]
    hT = p.sbuf("hTs", [128, KC, TL], BF16)
    for k in range(0, KC, 4):
        p.dma("sp", hT[:, k:k + 4, :], hT_d[:, k:k + 4, :], writes=[("hT", k)])
    allh = [("hT", k) for k in range(0, KC, 4)]
    s5d = p.sbuf("s5d", [128, 4]); bgT = p.sbuf("bgT", [128, 48])
    p.dma("pool", s5d[:], s5d_d, writes=["s5d"]); p.dma("pool", bgT[:], bgT_d, writes=["bgT"])
    feats = [p.sbuf("feat%d" % i, [128, 4, TL], BF16) for i in range(3)]
    yf32 = p.sbuf("yf32", [128, 4, TL]); ybf = p.sbuf("ybf", [128, 4, TL], BF16)
    st = [p.sbuf("st%d" % i, [128, TL]) for i in range(3)]
    wb32 = p.sbuf("wb32", [128, 4, 512]); wgl = p.sbuf("wgl", [128, 4, 512], BF16)
    p.dma("pool", wb32[:], wglu_d.rearrange("(c p) n -> p c n", p=128), writes=["wb32"])
    cp(p, "pool", wgl[:], wb32[:], ["wb32"], ["wgl"])
    for c in range(4):
        rows = slice(c * 128, (c + 1) * 128)
        p.dma("sp", st[0][:], ysf_d[rows, :], writes=[("st", 0)])
        p.dma("sp", st[1][:], ysb_d[rows, :], writes=[("st", 1)])
        p.dma("sp", st[2][:], uT_d[rows, :], writes=[("st", 2)])
        tt(p, "pool", st[0][:], st[0][:], st[1][:], ALU.add, [("st", 0), ("st", 1)], [("st", 0)])
        stt(p, "dve", st[0][:], st[2][:], s5d[:, c:c + 1], st[0][:], ALU.mult, ALU.add, [("st", 0), ("st", 2), "s5d"], [("st", 0)])
        act(p, yf32[:, c, :], st[0][:], AF.Gelu_apprx_tanh, [("st", 0)], [("yf32", c)])
        cp(p, "pool", ybf[:, c, :], yf32[:, c, :], [("yf32", c)], [("ybf", c)])
    sig = p.sbuf("sig", [128, 512])
    ally = [("ybf", c) for c in range(4)]
    for c2 in range(4):
        for bi, (t0, tn) in enumerate(TBLK):
            b = (c2 * 3 + bi) % 2
            for c in range(4):
                mm(p, P[b][:, 0:tn], wgl[:, c, c2 * 128:(c2 + 1) * 128], ybf[:, c, t0:t0 + tn], c == 0, c == 3, ["wgl"] + ally, [("ps", b)])
            act(p, sig[:, 0:tn], P[b][:, 0:tn], AF.Sigmoid, [("ps", b)], ["sig"])
            tt(p, "dve", feats[0][:, c2, t0:t0 + tn], yf32[:, c2, t0:t0 + tn], sig[:, 0:tn], ALU.mult, ["sig", ("yf32", c2)], [("feat", 0)])
    for fi, src in ((1, yf_d), (2, ysgu_d)):
        for c in range(4):
            s = (fi * 4 + c) % 3
            p.dma("sp", st[s][:], src[c * 128:(c + 1) * 128, :], writes=[("st", s)])
            cp(p, "pool" if c % 2 else "dve", feats[fi][:, c, :], st[s][:], [("st", s)], [("feat", fi)])
    wst = [p.sbuf("wst%d" % i, [128, KC, 256]) for i in range(2)]
    wgb = p.sbuf("wgb", [128, KC, 512], BF16)
    wbb = p.sbuf("wbb", [128, 4, 512], BF16)
    merged = yf32
    _m = p.sbuf("mTs0", [128, 4, TL], BF16)
    mTs = [_m, _m]
    gate = [p.sbuf("gate%d" % i, [128, 512]) for i in range(2)]
    gtmp = [p.sbuf("gtmp%d" % i, [128, 512]) for i in range(2)]
    n = 0
    for dq in range(4):
        for kbr in range(3):
            col0 = kbr * D + dq * 512
            for hf in range(2):
                p.dma("sp", wst[hf][:], wgate_d[:, col0 + hf * 256: col0 + (hf + 1) * 256].rearrange("(k p) c -> p k c", p=128), writes=[("wst", hf)])
                cp(p, "pool" if hf else "dve", wgb[:, :, hf * 256:(hf + 1) * 256], wst[hf][:], [("wst", hf)], ["wgb"])
            p.dma("pool", wb32[:], wbr_d[kbr, :, dq * 512:(dq + 1) * 512].rearrange("(c p) n -> p c n", p=128), writes=["wb32"])
            cp(p, "pool", wbb[:], wb32[:], ["wb32"], ["wbb"])
            for dl in range(4):
                dc = dq * 4 + dl
                for bi, (t0, tn) in enumerate(TBLK):
                    bg = (n % 2) * 2; bb_ = bg + 1
                    for k in range(KC):
                        mm(p, P[bg][:, 0:tn], wgb[:, k, dl * 128:(dl + 1) * 128], hT[:, k, t0:t0 + tn], k == 0, k == KC - 1, ["wgb"] + allh, [("ps", bg)])
                    for c in range(4):
                        mm(p, P[bb_][:, 0:tn], wbb[:, c, dl * 128:(dl + 1) * 128], feats[kbr][:, c, t0:t0 + tn], c == 0, c == 3, ["wbb", ("feat", kbr)], [("ps", bb_)])
                    g_ = gate[n % 2]
                    act(p, g_[:, 0:tn], P[bg][:, 0:tn], AF.Sigmoid, [("ps", bg)], [("gate", n % 2)], bias=bgT[:, kbr * 16 + dc: kbr * 16 + dc + 1])
                    mk = ("merged", dl, bi)
                    if kbr == 0:
                        tt(p, "dve", merged[:, dl, t0:t0 + tn], P[bb_][:, 0:tn], g_[:, 0:tn], ALU.mult, [("ps", bb_), ("gate", n % 2)], [mk])
                    else:
                        gt = gtmp[n % 2]
                        tt(p, "dve", gt[:, 0:tn], P[bb_][:, 0:tn], g_[:, 0:tn], ALU.mult, [("ps", bb_), ("gate", n % 2)], [("gtmp", n % 2)])
                        if kbr == 1:
                            tt(p, "pool", merged[:, dl, t0:t0 + tn], merged[:, dl, t0:t0 + tn], gt[:, 0:tn], ALU.add, [("gtmp", n % 2), mk], [mk])
                        else:
                            tt(p, "pool", mTs[dq % 2][:, dl, t0:t0 + tn], merged[:, dl, t0:t0 + tn], gt[:, 0:tn], ALU.add, [("gtmp", n % 2), mk], [("mTs", 0)])
                    n += 1
        p.dma("pool", mT_o[:, dq * 4:(dq + 1) * 4, :], mTs[0][:], reads=[("mTs", 0)], key=("mTo", 0))
    p.emit()
    return p


def build_posta2(nc):
    p = Prog(nc)
    x_d = dram_in(nc, "x", [TL, D])
    mT_d = dram_in(nc, "mT", [128, KC, TL], BF16)
    c2T = dram_in(nc, "c2T", [128, 32])
    wmod = dram_in(nc, "wmod", [D, 6144])
    bmodT = dram_in(nc, "bmodT", [128, 48])
    g2nT = dram_in(nc, "g2nT", [128, KC])
    wout_d = dram_in(nc, "wout", [D, D])
    rw_d = dram_in(nc, "rw", [D, 32])
    rb_d = dram_in(nc, "rb", [1, 32])
    x1_o = dram_out(nc, "x1", [TL, D])
    h2T_o = dram_out(nc, "h2T", [128, KC, TL])
    rwt_o = dram_out(nc, "rwt", [TL, 32])

    P = [p.psum("ps%d" % i, [128, 512]) for i in range(8)]
    ident, io, pc = make_ident(p)
    wst = [p.sbuf("wst%d" % i, [128, KC, 256]) for i in range(2)]
    modT = adaln_T(p, P, c2T, wmod, bmodT, 48, wts=wst, wkey="wst")
    g2n = p.sbuf("g2n", [128, KC])
    p.dma("pool", g2n[:], g2nT, writes=["g2n"])
    A2 = p.sbuf("A2", [128, 2, KC])
    for r in range(2):
        stt(p, "dve", A2[:, r, :], modT[:, 32:48, r], 1.0, g2n[:], ALU.add, ALU.mult, ["modT", "g2n"], ["A2"])
    ones = p.sbuf("ones", [128, 128])
    memset(p, "dve", ones[:], 1.0, ["ones"])
    g1b = p.sbuf("g1b", [128, 2, D])
    dg = [p.sbuf("dg%d" % i, [128, 128]) for i in range(2)]
    for r in range(2):
        for dc in range(KC):
            s = dc % 2
            ts(p, "dve", dg[s][:], ident[:], modT[:, dc, r:r + 1], ALU.mult, ["ident", "modT"], [("dg", s)])
            b = (dc // 4) % 2
            mm(p, P[b][:, (dc % 4) * 128:(dc % 4 + 1) * 128], ones[:], dg[s][:], True, True, ["ones", ("dg", s)], [("ps", b)])
            if dc % 4 == 3:
                cp(p, "act", g1b[:, r, (dc // 4) * 512:(dc // 4 + 1) * 512], P[b][:], [("ps", b)], ["g1b"])
    rw = p.sbuf("rws", [128, KC, 32]); rbb = p.sbuf("rbb", [128, 32])
    p.dma("pool", rw[:], rw_d.rearrange("(k p) e -> p k e", p=128), writes=["rw"])
    p.dma("pool", rbb[:], rb_d.partition_broadcast(128), writes=["rbb"])
    mT = p.sbuf("mTs", [128, KC, TL], BF16)
    for k in range(0, KC, 4):
        p.dma("sp", mT[:, k:k + 4, :], mT_d[:, k:k + 4, :], writes=[("mT", k)])
    allm = [("mT", k) for k in range(0, KC, 4)]
    wo = p.sbuf("wo", [128, KC, D], BF16)
    for t in range(8):
        s = t % 2
        p.dma("sp", wst[s][:], wout_d[:, t * 256:(t + 1) * 256].rearrange("(k p) c -> p k c", p=128), writes=[("wst", s)])
        cp(p, "pool" if s else "dve", wo[:, :, t * 256:(t + 1) * 256], wst[s][:], [("wst", s)], [("wo", t // 2)])
    xb = [p.sbuf("xb%d" % i, [128, D]) for i in range(2)]
    x1b = p.sbuf("x1b", [128, D])
    junk = p.sbuf("junk", [128, D], BF16)
    h2t = [p.sbuf("h2t%d" % i, [128, KC, 128]) for i in range(2)]
    ssq = p.sbuf("ssq", [128, NT]); rs = p.sbuf("rs", [128, NT])
    otmp = [p.sbuf("otmp%d" % i, [128, 512]) for i in range(2)]
    lg = p.sbuf("lg", [128, 32]); top8 = p.sbuf("top8", [128, 8]); msk = p.sbuf("msk", [128, 32])
    nmx = p.sbuf("nmx", [128, 1]); ex = p.sbuf("ex", [128, 32]); se = p.sbuf("se", [128, 1]); rwo = [p.sbuf("rwo%d" % i, [128, 32]) for i in range(2)]
    ev = 0
    for t in range(NT):
        r = 0 if t < 8 else 1
        s = t % 2
        xt = xb[s]
        p.dma("sp", xt[:], x_d[t * 128:(t + 1) * 128, :], writes=[("xb", s)])
        for cb in range(4):
            b = (t * 4 + cb) % 4
            for k in range(KC):
                mm(p, P[b][:], mT[:, k, t * 128:(t + 1) * 128], wo[:, k, cb * 512:(cb + 1) * 512], k == 0, k == KC - 1, allm + [("wo", cb)], [("ps", b)])
            ot = otmp[cb % 2]
            tt(p, "dve", ot[:], P[b][:], g1b[:, r, cb * 512:(cb + 1) * 512], ALU.mult, [("ps", b), "g1b"], [("otmp", cb % 2)])
            tt(p, "pool", x1b[:, cb * 512:(cb + 1) * 512], xt[:, cb * 512:(cb + 1) * 512], ot[:], ALU.add, [("otmp", cb % 2), ("xb", s)], ["x1b"])
        p.dma("pool", x1_o[t * 128:(t + 1) * 128, :], x1b[:], reads=["x1b"], key="x1o")
        act(p, junk[:], x1b[:], AF.Square, ["x1b"], ["junk", ("ssq", t)], accum_out=ssq[:, t:t + 1])
        act(p, rs[:, t:t + 1], ssq[:, t:t + 1], AF.Sqrt, [("ssq", t)], [("rs", t)], scale=1.0 / D, bias=EPS)
        recip(p, rs[:, t:t + 1], rs[:, t:t + 1], [("rs", t)], [("rs", t)])
        ts(p, "pool", xt[:], x1b[:], rs[:, t:t + 1], ALU.mult, ["x1b", ("rs", t), ("xb", s)], [("xb", s)])
        h2 = h2t[s]
        for kq in range(4):
            b = 4 + (t * 4 + kq) % 2
            for j in range(4):
                k = kq * 4 + j
                tr(p, P[b][:, j * 128:(j + 1) * 128], xt[:, k * 128:(k + 1) * 128], ident[:], [("xb", s)], [("ps", b)])
            for j in range(4):
                k = kq * 4 + j
                i_ = P[b][:, j * 128:(j + 1) * 128]
                if ev % 2 == 0:
                    ts(p, "dve", h2[:, k, :], i_, A2[:, r, k:k + 1], ALU.mult, [("ps", b), "A2", "modT"], [("h2t", s)],
                       s2=modT[:, 16 + k, r:r + 1], op1=ALU.add)
                else:
                    act(p, h2[:, k, :], i_, AF.Identity, [("ps", b), "A2", "modT"], [("h2t", s)],
                        scale=A2[:, r, k:k + 1], bias=modT[:, 16 + k, r:r + 1])
                ev += 1
        p.dma("pool", h2T_o[:, :, t * 128:(t + 1) * 128], h2[:], reads=[("h2t", s)], key=("h2o", s))
        for k in range(KC):
            mm(p, P[6][:, 0:32], h2[:, k, :], rw[:, k, :], k == 0, k == KC - 1, [("h2t", s), "rw"], [("ps", 6)])
        tt(p, "dve", lg[:], P[6][:, 0:32], rbb[:], ALU.add, [("ps", 6), "rbb"], ["lg"])
        p.op("dve", lambda e: nc.vector.max(out=top8[:], in_=lg[:]), ["lg"], ["top8"])
        ts(p, "dve", msk[:], lg[:], top8[:, 3:4], ALU.is_ge, ["lg", "top8"], ["msk"])
        ts(p, "dve", nmx[:], top8[:, 0:1], -1.0, ALU.mult, ["top8"], ["nmx"])
        act(p, ex[:], lg[:], AF.Exp, ["lg", "nmx"], ["ex"], bias=nmx[:, 0:1])
        tt(p, "dve", ex[:], ex[:], msk[:], ALU.mult, ["ex", "msk"], ["ex"])
        p.op("dve", lambda e: nc.vector.reduce_sum(out=se[:], in_=ex[:], axis=AX.X), ["ex"], ["se"])
        recip(p, se[:], se[:], ["se"], ["se"])
        ro = rwo[s]
        ts(p, "dve", ro[:], ex[:], se[:, 0:1], ALU.mult, ["ex", "se"], [("rwo", s)])
        p.dma("pool", rwt_o[t * 128:(t + 1) * 128, :], ro[:], reads=[("rwo", s)], key=("rwo", s))
    p.emit()
    return p


CAP = 4096
SCH = 1024
NBC = SCH // 128
NB = CAP // 128
NTT = NTOK // 128
NROW = NTOK + CAP
SW_LIMIT = 7.0
SW_ALPHA = 1.702
SBLK = [(0, 512), (512, 512)]


def build_moe(nc):
    p = Prog(nc)
    h2p = dram_in(nc, "h2p", [NROW, D])
    rwT_d = dram_in(nc, "rwT", [128, NTT * 4])
    wup_d = dram_in(nc, "wup", [4, D, 2 * D])
    bupT_d = dram_in(nc, "bupT", [128, 4 * 32])
    wdn_d = dram_in(nc, "wdn", [4, D, D])
    bdn_d = dram_in(nc, "bdn", [4, D])
    yp_o = [dram_out(nc, "yp%d" % i, [NROW, 512]) for i in range(4)]

    P = [p.psum("ps%d" % i, [128, 512]) for i in range(8)]
    ident, io, pc = make_ident(p)

    zt = p.sbuf("zt", [128, 512])
    memset(p, "dve", zt[:], 0.0, ["zt"])
    nz = 0
    yz_keys = []
    for i in range(4):
        for r0 in range(0, NROW, 128 * 18):
            nr = min(128 * 18, NROW - r0)
            k_ = ("yz", nz); yz_keys.append(k_); nz += 1
            p.dma("pool", yp_o[i][r0:r0 + nr, :].rearrange("(a p) c -> p a c", p=128), zt[:].unsqueeze(1).to_broadcast([128, nr // 128, 512]),
                  reads=["zt"], writes=[k_], key="yz")

    rw = p.sbuf("rw", [128, NTT, 4]); msk = p.sbuf("msk", [128, NTT, 4]); rank = p.sbuf("rank", [128, NTT, 4])
    p.dma("sp", rw[:].rearrange("p a b -> p (a b)"), rwT_d, writes=["rw"])
    ts(p, "dve", msk[:], rw[:], 0.0, ALU.is_gt, ["rw"], ["msk"])
    ones = p.sbuf("ones", [128, 128]); Ls = p.sbuf("Ls", [128, 128])
    memset(p, "dve", ones[:], 1.0, ["ones"])
    ts(p, "dve", Ls[:], io[:], pc[:, 0:1], ALU.is_gt, ["io_id", "pc_id"], ["Ls"])
    for t in range(NTT):
        mm(p, P[0][:, t * 4:(t + 1) * 4], Ls[:], msk[:, t, :], True, True, ["Ls", "msk"], [("ps", 0)])
    mm(p, P[1][:, 0:NTT * 4], ones[:], msk[:].rearrange("p a b -> p (a b)"), True, True, ["ones", "msk"], [("ps", 1)])
    tot = p.sbuf("tot", [128, NTT, 4]); cum = p.sbuf("cum", [128, NTT, 4])
    cp(p, "dve", tot[:].rearrange("p a b -> p (a b)"), P[1][:, 0:NTT * 4], [("ps", 1)], ["tot"])
    for e in range(4):
        p.op("dve", lambda e_, e=e: nc.vector.tensor_tensor_scan(out=cum[:, :, e], data0=ones[:, 0:NTT], data1=tot[:, :, e], initial=0.0,
                                                                 op0=ALU.mult, op1=ALU.add), ["tot", "ones"], ["cum"])
    tt(p, "dve", cum[:], cum[:], tot[:], ALU.subtract, ["cum", "tot"], ["cum"])
    tt(p, "dve", rank[:].rearrange("p a b -> p (a b)"), P[0][:, 0:NTT * 4], cum[:].rearrange("p a b -> p (a b)"), ALU.add, [("ps", 0), "cum"], ["rank"])
    stt(p, "dve", rank[:], rank[:], 1.0, msk[:], ALU.add, ALU.mult, ["rank", "msk"], ["rank"])
    ts(p, "dve", rank[:], rank[:], -1.0, ALU.add, ["rank"], ["rank"])
    tokc = p.sbuf("tokc", [128, NTT, 4, 8], BF16)
    tilev = p.sbuf("tilev", [128, NTT])
    rwh = p.sbuf("rwh", [128, NTT, 4], BF16); rwh32 = p.sbuf("rwh32", [128, NTT, 4]); rwl = p.sbuf("rwl", [128, NTT, 4])
    p.op("pool", lambda e: nc.gpsimd.iota(tilev[:], pattern=[[1, NTT]], base=0, channel_multiplier=0, allow_small_or_imprecise_dtypes=True), [], ["tilev"])
    memset(p, "dve", tokc[:], 0.0, ["tokc"])
    cp(p, "dve", rwh[:], rw[:], ["rw"], ["rwh"])
    cp(p, "dve", rwh32[:], rwh[:], ["rwh"], ["rwh32"])
    tt(p, "dve", rwl[:], rw[:], rwh32[:], ALU.subtract, ["rw", "rwh32"], ["rwl"])
    for e in range(4):
        cp(p, "dve", tokc[:, :, e, 0], tilev[:], ["tilev", "tokc"], ["tokc"])
        cp(p, "dve", tokc[:, :, e, 1], pc[:, 0:1].to_broadcast([128, NTT]), ["pc_id", "tokc"], ["tokc"])
        cp(p, "dve", tokc[:, :, e, 2], ones[:, 0:NTT], ["ones", "tokc"], ["tokc"])
        cp(p, "dve", tokc[:, :, e, 3], rwh[:, :, e], ["rwh", "tokc"], ["tokc"])
        cp(p, "dve", tokc[:, :, e, 4], rwl[:, :, e], ["rwl", "tokc"], ["tokc"])
    OHH = CAP // 2
    iot = p.sbuf("iot", [128, OHH])
    p.op("pool", lambda e: nc.gpsimd.iota(iot[:], pattern=[[1, OHH]], base=0, channel_multiplier=0, allow_small_or_imprecise_dtypes=True), [], ["iot"])
    rank2 = p.sbuf("rank2", [128, NTT, 4])
    ts(p, "dve", rank2[:], rank[:], -float(OHH), ALU.add, ["rank"], ["rank2"])
    _oh = p.sbuf("OH0", [128, CAP], BF16)
    OH = [_oh, _oh]
    EC = NB * 8
    zl = p.sbuf("zl", [128, 128], BF16); zr = p.sbuf("zr", [128, 2 * EC], BF16)
    memset(p, "dve", zl[:], 0.0, ["zl"]); memset(p, "dve", zr[:], 0.0, ["zr"])
    for bk_ in (6, 7):
        mm(p, P[bk_][:, 0:2 * EC], zl[:], zr[:], True, False, ["zl", "zr"], [("ps", bk_)])
    n = 0
    for t in range(NTT):
        for e in range(4):
            oh = OH[0]
            ts(p, "dve", oh[:, 0:OHH], iot[:], rank[:, t, e:e + 1], ALU.is_equal, ["iot", "rank"], [("OH", 0)])
            ts(p, "pool", oh[:, OHH:CAP], iot[:], rank2[:, t, e:e + 1], ALU.is_equal, ["iot", "rank2"], [("OH", 1)])
            for b in range(NB):
                c0 = (e % 2) * EC + b * 8
                mm(p, P[6 + e // 2][:, c0:c0 + 5], oh[:, b * 128:(b + 1) * 128], tokc[:, t, e, 0:5], False, (t == NTT - 1 and b == NB - 1),
                   [("OH", 0), ("OH", 1), "tokc"], [("ps", 6 + e // 2)])
            n += 1
    sacc = p.sbuf("sacc", [128, 4, NB, 8])
    for hh_ in range(2):
        cp(p, "dve", sacc[:, 2 * hh_:2 * hh_ + 2, :, :].rearrange("p a b c -> p (a b c)"), P[6 + hh_][:, 0:2 * EC], [("ps", 6 + hh_)], ["sacc"])
    padv = p.sbuf("padv", [128, NB])
    p.op("pool", lambda e: nc.gpsimd.iota(padv[:], pattern=[[128, NB]], base=NTOK, channel_multiplier=1, allow_small_or_imprecise_dtypes=True), [], ["padv"])
    idxf = p.sbuf("idxf", [128, 4, NB]); t2 = p.sbuf("t2", [128, 4, NB]); idx = p.sbuf("idx", [128, 4, NB], I32); wsl = p.sbuf("wsl", [128, 4, NB])
    for e in range(4):
        stt(p, "dve", idxf[:, e, :], sacc[:, e, :, 0], 128.0, sacc[:, e, :, 1], ALU.mult, ALU.add, ["sacc"], ["idxf"])
        tt(p, "dve", t2[:, e, :], sacc[:, e, :, 2], padv[:], ALU.mult, ["sacc", "padv"], ["t2"])
        tt(p, "dve", t2[:, e, :], padv[:], t2[:, e, :], ALU.subtract, ["padv", "t2"], ["t2"])
        tt(p, "dve", idxf[:, e, :], idxf[:, e, :], t2[:, e, :], ALU.add, ["idxf", "t2"], ["idxf"])
        tt(p, "dve", wsl[:, e, :], sacc[:, e, :, 3], sacc[:, e, :, 4], ALU.add, ["sacc"], ["wsl"])
    cp(p, "dve", idx[:], idxf[:], ["idxf"], ["idx"])

    xbT = p.sbuf("xbT", [128, KC, SCH], BF16)
    actT = p.sbuf("actT", [128, KC, SCH], BF16)
    xg = [p.sbuf("xg%d" % i, [128, D]) for i in range(2)]
    wst = [p.sbuf("wst%d" % i, [128, KC, 256]) for i in range(2)]
    wbf = [[p.sbuf("wbf%d_%d" % (i, j), [128, KC, 256], BF16) for j in range(2)] for i in range(2)]
    bupT = p.sbuf("bupT", [128, 4, 32])
    p.dma("sp", bupT[:].rearrange("p a b -> p (a b)"), bupT_d, writes=["bupT"])
    bdr = p.sbuf("bdr", [1, D], BF16); ones1 = p.sbuf("ones1", [1, 128], BF16)
    memset(p, "dve", ones1[:], 1.0, ["ones1"])
    G = {nm: [p.sbuf("g_%s%d" % (nm, i), [128, 512]) for i in range(2)] for nm in ("g", "sg", "u")}
    NY = 3
    ybs = [p.sbuf("ybs%d" % i, [128, 512]) for i in range(NY)]
    ng = 0; ne = 0; ny = 0; nwu = 0
    for e in range(4):
        p.dma("pool", bdr[:], bdn_d[e:e + 1, :], writes=["bdr"])
        for ch in range(CAP // SCH):
            for bl in range(NBC):
                b = ch * NBC + bl
                xs = xg[ng % 2]
                p.op("pool", lambda e_, xs=xs, e=e, b=b: nc.gpsimd.indirect_dma_start(
                    out=xs[:], out_offset=None, in_=h2p, in_offset=bass.IndirectOffsetOnAxis(ap=idx[:, e, b:b + 1], axis=0)),
                    ["idx"], [("xg", ng % 2)], dma=True, key=("dma", ("xg", ng % 2)))
                for kq in range(4):
                    bk = kq % 2
                    for j in range(4):
                        k = kq * 4 + j
                        tr(p, P[bk][:, j * 128:(j + 1) * 128], xs[:, k * 128:(k + 1) * 128], ident[:], [("xg", ng % 2)], [("ps", bk)])
                    cp(p, "act" if kq % 2 else "dve", xbT[:, kq * 4:(kq + 1) * 4, bl * 128:(bl + 1) * 128],
                       P[bk][:].rearrange("p (j s) -> p j s", j=4), [("ps", bk)], [("xbT", bl)])
                ng += 1
            allx = [("xbT", bl) for bl in range(NBC)]
            for fq in range(8):
                par = nwu % 2
                for hf in range(2):
                    c0 = hf * D + fq * 256
                    p.dma("sp", wst[hf][:], wup_d[e, :, c0:c0 + 256].rearrange("(k p) c -> p k c", p=128), writes=[("wst", hf)])
                    cp(p, "dve", wbf[hf][par][:], wst[hf][:], [("wst", hf)], [("wbf", hf, par)])
                nwu += 1
                for j in range(2):
                    fc = fq * 2 + j
                    for bi, (s0, sn) in enumerate(SBLK):
                        bg = 2 + (ne % 2) * 2; bu = bg + 1
                        for k in range(KC):
                            mm(p, P[bg][:, 0:sn], wbf[0][par][:, k, j * 128:(j + 1) * 128], xbT[:, k, s0:s0 + sn], k == 0, k == KC - 1, [("wbf", 0, par)] + allx, [("ps", bg)])
                        for k in range(KC):
                            mm(p, P[bu][:, 0:sn], wbf[1][par][:, k, j * 128:(j + 1) * 128], xbT[:, k, s0:s0 + sn], k == 0, k == KC - 1, [("wbf", 1, par)] + allx, [("ps", bu)])
                        w_ = ne % 2
                        g_, sg_, u_ = G["g"][w_][:, 0:sn], G["sg"][w_][:, 0:sn], G["u"][w_][:, 0:sn]
                        ts(p, "dve", g_, P[bg][:, 0:sn], bupT[:, e, fc:fc + 1], ALU.add, [("ps", bg), "bupT"], [("g", w_)], s2=SW_LIMIT, op1=ALU.min)
                        act(p, sg_, g_, AF.Sigmoid, [("g", w_)], [("sg", w_)], scale=SW_ALPHA)
                        ts(p, "dve", u_, P[bu][:, 0:sn], bupT[:, e, 16 + fc:17 + fc], ALU.add, [("ps", bu), "bupT"], [("u", w_)], s2=SW_LIMIT, op1=ALU.min)
                        ts(p, "dve", u_, u_, -SW_LIMIT, ALU.max, [("u", w_)], [("u", w_)], s2=1.0, op1=ALU.add)
                        tt(p, "dve", g_, g_, sg_, ALU.mult, [("g", w_), ("sg", w_)], [("g", w_)])
                        tt(p, "dve", actT[:, fc, s0:s0 + sn], g_, u_, ALU.mult, [("g", w_), ("u", w_)], [("actT", bi)])
                        ne += 1
            alla = [("actT", bi) for bi in range(len(SBLK))]
            for db in range(4):
                par = nwu % 2
                for hf in range(2):
                    c0 = db * 512 + hf * 256
                    p.dma("sp", wst[hf][:], wdn_d[e, :, c0:c0 + 256].rearrange("(k p) c -> p k c", p=128), writes=[("wst", hf)])
                    cp(p, "dve", wbf[hf][par][:], wst[hf][:], [("wst", hf)], [("wbf", hf, par)])
                nwu += 1
                for bl in range(NBC):
                    b = ch * NBC + bl
                    bk = ny % 2
                    for hf in range(2):
                        c0 = db * 512 + hf * 256
                        mm(p, P[bk][:, hf * 256:(hf + 1) * 256], ones1[:], bdr[:, c0:c0 + 256], True, False, ["ones1", "bdr"], [("ps", bk)])
                        for k in range(KC):
                            mm(p, P[bk][:, hf * 256:(hf + 1) * 256], actT[:, k, bl * 128:(bl + 1) * 128], wbf[hf][par][:, k, :], False, k == KC - 1,
                               [("wbf", hf, par)] + alla, [("ps", bk)])
                    yi = ny % NY
                    y_ = ybs[yi]
                    act(p, y_[:], P[bk][:], AF.Copy, [("ps", bk), "wsl"], [("ybs", yi)], scale=wsl[:, e, b:b + 1])
                    rd = [("ybs", yi), "idx"]
                    if e == 0:
                        rd += yz_keys
                    else:
                        rd += [("ya", db, e - 1, b2) for b2 in range(NB)]
                    p.op("pool", lambda e_, y_=y_, e=e, b=b, db=db: nc.gpsimd.indirect_dma_start(
                        out=yp_o[db], out_offset=bass.IndirectOffsetOnAxis(ap=idx[:, e, b:b + 1], axis=0), in_=y_[:], in_offset=None,
                        compute_op=ALU.add), rd, [("ya", db, e, b)], dma=True, key=("dma", ("yacc", db)))
                    ny += 1
    p.emit()
    return p


def build_comb(nc):
    p = Prog(nc)
    x1_d = dram_in(nc, "x1", [TL, D])
    yp_d = dram_in(nc, "yps", [NCORES, TL, D])
    c2T = dram_in(nc, "c2T", [128, 32])
    wmod = dram_in(nc, "wmod", [D, D])
    bmodT = dram_in(nc, "bmodT", [128, KC])
    fg_d = dram_in(nc, "fg", [1, D])
    x2_o = dram_out(nc, "x2", [TL, D])
    xn_o = dram_out(nc, "xn", [TL, D])
    P = [p.psum("ps%d" % i, [128, 512]) for i in range(8)]
    ident, io, pc = make_ident(p)
    modT = adaln_T(p, P, c2T, wmod, bmodT, 16)
    ones = p.sbuf("ones", [128, 128])
    memset(p, "dve", ones[:], 1.0, ["ones"])
    g2b = p.sbuf("g2b", [128, 2, D])
    dg = [p.sbuf("dg%d" % i, [128, 128]) for i in range(2)]
    for r in range(2):
        for dc in range(KC):
            s = dc % 2
            ts(p, "dve", dg[s][:], ident[:], modT[:, dc, r:r + 1], ALU.mult, ["ident", "modT"], [("dg", s)])
            b = (dc // 4) % 2
            mm(p, P[b][:, (dc % 4) * 128:(dc % 4 + 1) * 128], ones[:], dg[s][:], True, True, ["ones", ("dg", s)], [("ps", b)])
            if dc % 4 == 3:
                cp(p, "act", g2b[:, r, (dc // 4) * 512:(dc // 4 + 1) * 512], P[b][:], [("ps", b)], ["g2b"])
    fgb = p.sbuf("fgb", [128, D])
    p.dma("pool", fgb[:], fg_d.partition_broadcast(128), writes=["fgb"])
    xb = [p.sbuf("xb%d" % i, [128, D]) for i in range(2)]
    yb = [p.sbuf("yb%d" % i, [128, 4, D]) for i in range(2)]
    xo = [p.sbuf("xo%d" % i, [128, D]) for i in range(2)]
    junk = p.sbuf("junk", [128, D], BF16)
    ssq = p.sbuf("ssq", [128, NT]); rs = p.sbuf("rs", [128, NT])
    for t in range(NT):
        r = 0 if t < 8 else 1
        s = t % 2
        rows = slice(t * 128, (t + 1) * 128)
        p.dma("sp", xb[s][:], x1_d[rows, :], writes=[("xb", s)])
        for hv in range(2):
            p.dma("sp", yb[hv][:], yp_d[hv * 4:(hv + 1) * 4, rows, :].rearrange("c p d -> p c d"), writes=[("yb", hv)])
        tt(p, "dve", yb[0][:, 0:2, :], yb[0][:, 0:2, :], yb[0][:, 2:4, :], ALU.add, [("yb", 0)], [("yb", 0)])
        tt(p, "pool", yb[1][:, 0:2, :], yb[1][:, 0:2, :], yb[1][:, 2:4, :], ALU.add, [("yb", 1)], [("yb", 1)])
        tt(p, "dve", yb[0][:, 0:2, :], yb[0][:, 0:2, :], yb[1][:, 0:2, :], ALU.add, [("yb", 0), ("yb", 1)], [("yb", 0)])
        tt(p, "pool", yb[0][:, 0, :], yb[0][:, 0, :], yb[0][:, 1, :], ALU.add, [("yb", 0)], [("yb", 0)])
        tt(p, "dve", yb[0][:, 0, :], yb[0][:, 0, :], g2b[:, r, :], ALU.mult, [("yb", 0), "g2b"], [("yb", 0)])
        tt(p, "pool", xb[s][:], xb[s][:], yb[0][:, 0, :], ALU.add, [("yb", 0), ("xb", s)], [("xb", s)])
        p.dma("pool", x2_o[rows, :], xb[s][:], reads=[("xb", s)], key=("x2o", s))
        act(p, junk[:], xb[s][:], AF.Square, [("xb", s)], ["junk", ("ssq", t)], accum_out=ssq[:, t:t + 1])
        act(p, rs[:, t:t + 1], ssq[:, t:t + 1], AF.Sqrt, [("ssq", t)], [("rs", t)], scale=1.0 / D, bias=EPS)
        recip(p, rs[:, t:t + 1], rs[:, t:t + 1], [("rs", t)], [("rs", t)])
        stt(p, "dve", xo[s][:], xb[s][:], rs[:, t:t + 1], fgb[:], ALU.mult, ALU.mult, [("xb", s), ("rs", t), "fgb"], [("xo", s)])
        p.dma("pool", xn_o[rows, :], xo[s][:], reads=[("xo", s)], key=("xno", s))
    p.emit()
    return p


def fm(v, nch):
    return np.ascontiguousarray(np.asarray(v).reshape(nch, 128).T)

def c2T_of(c, c_ctx):
    c2 = np.stack([np.asarray(c).reshape(-1), np.asarray(c_ctx).reshape(-1)])
    return np.ascontiguousarray(c2.reshape(2, 16, 128).transpose(2, 1, 0).reshape(128, 32))


def s5_core_inputs(cid, are, aim, ldt, bre, bim, cre, cim):
    spar = np.zeros((128, 4, 3), np.float32)
    bre_b = np.zeros((128, 4, 32), np.float32); bim_b = np.zeros((128, 4, 32), np.float32)
    cre_b = np.zeros((128, 4, 64), np.float32); cim_b = np.zeros((128, 4, 64), np.float32)
    for d in range(2):
        for gp in range(2):
            ti = d * 2 + gp
            for g2 in range(2):
                G = 4 * cid + gp * 2 + g2
                ps = slice(g2 * 64, g2 * 64 + 64)
                spar[ps, ti, 0] = are[d, G]; spar[ps, ti, 1] = aim[d, G]; spar[ps, ti, 2] = ldt[d, G]
                bre_b[ps, ti, g2 * 16:(g2 + 1) * 16] = bre[d, G]
                bim_b[ps, ti, g2 * 16:(g2 + 1) * 16] = bim[d, G]
                c0 = gp * 32 + g2 * 16
                cre_b[ps, ti, c0:c0 + 16] = cre[d, G].T
                cim_b[ps, ti, c0:c0 + 16] = cim[d, G].T
    return dict(spar=spar, bre=bre_b, bim=bim_b, cre=cre_b, cim=cim_b)


def mix_inputs(cid, zs5T_lat, zs5T_ctx, zfT_lat, zfT_ctx, s5p):
    rows = slice(64 * cid, 64 * cid + 64)
    uf = np.concatenate([zs5T_ctx[rows], zs5T_lat[rows]], 1)
    ub = np.concatenate([zs5T_ctx[rows][:, ::-1], zs5T_lat[rows][:, ::-1]], 1)
    g, half = cid // 2, cid % 2
    kbv = np.tile(np.arange(8, dtype=np.float32)[None] + 8 * half, (128, 1)).astype(np.float32)
    d = dict(uf=np.ascontiguousarray(uf), ub=np.ascontiguousarray(ub),
             zfl=np.ascontiguousarray(zfT_lat[g * 128:(g + 1) * 128]), zfc=np.ascontiguousarray(zfT_ctx[g * 128:(g + 1) * 128]), kbv=kbv)
    d.update(s5_core_inputs(cid, *s5p))
    return d


def _run(build, in_maps):
    nc = bass.Bass("TRN2", target_bir_lowering=False)
    build(nc)
    res = run_bass_kernel_spmd(nc, in_maps, core_ids=list(range(NCORES)))
    return res.results


def _rows_from_T(hT):
    a = np.asarray(hT)
    return np.ascontiguousarray(a.transpose(2, 1, 0).reshape(a.shape[2], a.shape[1] * 128))


def kernel(x, c, ctx, c_ctx, w_mod, b_mod, norm1_g, norm2_g, w_in,
           s5_a_re, s5_a_im, s5_log_dt, s5_b_re, s5_b_im, s5_c_re, s5_c_im, s5_d, s5_w_glu,
           sgu_norm_g, sgu_w, sgu_b, w_branch, w_gate, b_gate, w_out,
           router_w, router_b, moe_w_up, moe_b_up, moe_w_down, moe_b_down, final_g):
    f32 = np.float32
    A = lambda a: np.ascontiguousarray(np.asarray(a, dtype=f32))
    xl = np.asarray(x, dtype=f32)[0]
    xc = np.asarray(ctx, dtype=f32)[0]
    c2T = c2T_of(np.asarray(c, dtype=f32), np.asarray(c_ctx, dtype=f32))
    xs = [np.concatenate([xl[TLAT * j:TLAT * (j + 1)], xc], 0) for j in range(NCORES)]
    depth = np.asarray(w_mod).shape[0]
    xn = None
    for L in range(depth):
        wm = np.asarray(w_mod[L], dtype=f32); bm = np.asarray(b_mod[L], dtype=f32)
        shared = dict(c2T=c2T, wmod=A(wm[:, :4096]), bmodT=fm(bm[:4096], 32), g1nT=fm(norm1_g[L], KC), w_in=A(w_in[L]),
                      gv=A(sgu_norm_g[L]).reshape(1, 512), sguw=A(sgu_w[L]), sgub=A(sgu_b[L]).reshape(1, 512))
        pre = _run(build_pre, [dict(shared, x=xs[j]) for j in range(NCORES)])
        zT = [np.asarray(pre[j]["zT"]) for j in range(NCORES)]
        zs5_lat = np.concatenate([zT[j][0:512, :TLAT] for j in range(NCORES)], 1)
        zf_lat = np.concatenate([zT[j][512:1024, :TLAT] for j in range(NCORES)], 1)
        zs5_ctx = zT[0][0:512, TLAT:]
        zf_ctx = zT[0][512:1024, TLAT:]
        s5p = [np.asarray(a[L], dtype=f32) for a in (s5_a_re, s5_a_im, s5_log_dt, s5_b_re, s5_b_im, s5_c_re, s5_c_im)]
        mix = _run(build_mix, [mix_inputs(j, zs5_lat, zs5_ctx, zf_lat, zf_ctx, s5p) for j in range(NCORES)])
        ysf_seq = np.concatenate([np.asarray(mix[j]["ysf"]) for j in range(NCORES)], 0)
        ysb_seq = np.concatenate([np.asarray(mix[j]["ysb"]) for j in range(NCORES)], 0)
        ysf_ctx, ysf_lat = ysf_seq[:, :CTX], ysf_seq[:, CTX:]
        ysb_ctx, ysb_lat = ysb_seq[:, :CTX][:, ::-1], ysb_seq[:, CTX:][:, ::-1]
        yf_lat = np.concatenate([np.concatenate([np.asarray(mix[2 * g + h]["yf"]) for h in range(2)], 1) for g in range(4)], 0)
        yf_ctx = np.concatenate([np.asarray(mix[2 * g]["yfc"]) for g in range(4)], 0)

        def tokT(lat, cx, j):
            return np.ascontiguousarray(np.concatenate([lat[:, TLAT * j:TLAT * (j + 1)], cx], 1))
        sh1 = dict(s5d=fm(s5_d[L], 4), wglu=A(s5_w_glu[L]), wbr=A(w_branch[L]), wgate=A(w_gate[L]), bgT=fm(b_gate[L], 48))
        p1 = _run(build_posta1, [dict(sh1, hT=np.asarray(pre[j]["hT"]), uT=np.ascontiguousarray(zT[j][0:512]),
                                     ysf=tokT(ysf_lat, ysf_ctx, j), ysb=tokT(ysb_lat, ysb_ctx, j), yf=tokT(yf_lat, yf_ctx, j),
                                     ysgu=np.asarray(pre[j]["ysgu"])) for j in range(NCORES)])
        sh2 = dict(c2T=c2T, wmod=A(wm[:, 4096:10240]), bmodT=fm(bm[4096:10240], 48), g2nT=fm(norm2_g[L], KC), wout=A(w_out[L]),
                   rw=A(router_w[L]), rb=A(router_b[L]).reshape(1, 32))
        p2 = _run(build_posta2, [dict(sh2, x=xs[j], mT=np.asarray(p1[j]["mT"])) for j in range(NCORES)])
        h2r = [_rows_from_T(p2[j]["h2T"]) for j in range(NCORES)]
        h2p = np.concatenate([h2r[j][:TLAT] for j in range(NCORES)] + [h2r[0][TLAT:], np.zeros((CAP, D), f32)], 0)
        rwa = np.concatenate([np.asarray(p2[j]["rwt"])[:TLAT] for j in range(NCORES)] + [np.asarray(p2[0]["rwt"])[TLAT:]], 0)
        moe_in = []
        for j in range(NCORES):
            es = slice(4 * j, 4 * j + 4)
            rwc = rwa[:, es]
            rwT = np.ascontiguousarray(rwc.reshape(NTT, 128, 4).transpose(1, 0, 2).reshape(128, NTT * 4))
            bup = np.asarray(moe_b_up[L][es], dtype=f32)
            bupT = np.ascontiguousarray(bup.reshape(4, 32, 128).transpose(2, 0, 1).reshape(128, 128))
            moe_in.append(dict(h2p=h2p, rwT=rwT, wup=A(moe_w_up[L][es]), bupT=bupT, wdn=A(moe_w_down[L][es]), bdn=A(moe_b_down[L][es])))
        mo = _run(build_moe, moe_in)
        del moe_in
        yparts = [np.concatenate([np.asarray(mo[j]["yp%d" % i]) for i in range(4)], 1) for j in range(NCORES)]
        shc = dict(c2T=c2T, wmod=A(wm[:, 10240:12288]), bmodT=fm(bm[10240:12288], KC), fg=A(final_g).reshape(1, D))
        cin = []
        for j in range(NCORES):
            yps = np.stack([np.concatenate([yparts[e][TLAT * j:TLAT * (j + 1)], yparts[e][SEQ:NTOK]], 0) for e in range(NCORES)], 0)
            cin.append(dict(shc, x1=np.asarray(p2[j]["x1"]), yps=yps))
        cb = _run(build_comb, cin)
        del cin
        xs = [np.asarray(cb[j]["x2"]) for j in range(NCORES)]
        xn = [np.asarray(cb[j]["xn"]) for j in range(NCORES)]
    out = np.concatenate([xn[j][:TLAT] for j in range(NCORES)], 0).reshape(1, SEQ, D).astype(np.float32)
    return out
```

```python
import contextlib
import numpy as np
import concourse.bass as bass
import concourse.mybir as mybir

F32 = mybir.dt.float32
BF16 = mybir.dt.bfloat16
I32 = mybir.dt.int32
U32 = mybir.dt.uint32
ALU = mybir.AluOpType
AF = mybir.ActivationFunctionType
AX = mybir.AxisListType

ENGS = ("pe", "act", "dve", "pool", "sp")


class _Op:
    __slots__ = ("eng", "fn", "deps", "dma", "key", "needs_inc", "count", "idx", "inc", "kind", "flag")

    def __init__(self, eng, fn, dma, key):
        self.eng = eng
        self.fn = fn
        self.deps = []
        self.dma = dma
        self.key = key
        self.needs_inc = False
        self.count = None
        self.inc = 16
        self.kind = None
        self.flag = None


class Prog:
    def __init__(self, nc):
        self.nc = nc
        self.streams = {e: [] for e in ENGS}
        self.res = {}
        self.stack = contextlib.ExitStack()
        self.dma_keys = {}
        self.nops = 0

    def sbuf(self, name, shape, dtype=F32):
        return self.stack.enter_context(self.nc.sbuf_tensor("sb_" + name, list(shape), dtype))

    def psum(self, name, shape, dtype=F32):
        return self.stack.enter_context(self.nc.psum_tensor("pp_" + name, list(shape), dtype))

    def op(self, eng, fn, reads=(), writes=(), dma=False, key=None):
        o = _Op(eng, fn, dma, key)
        o.idx = self.nops
        self.nops += 1
        deps = set()
        for k in reads:
            r = self.res.get(k)
            if r is None:
                r = self.res[k] = [None, []]
            if r[0] is not None:
                deps.add(r[0])
        for k in writes:
            r = self.res.get(k)
            if r is None:
                r = self.res[k] = [None, []]
            if r[0] is not None:
                deps.add(r[0])
            for rd in r[1]:
                deps.add(rd)
        for k in reads:
            self.res[k][1].append(o)
        for k in writes:
            self.res[k][0] = o
            self.res[k][1] = []
        for d in deps:
            if d is o:
                continue
            if (not d.dma) and (not dma) and d.eng == "pe" and eng == "pe":
                continue
            d.needs_inc = True
            o.deps.append(d)
        if dma:
            o.needs_inc = True
            assert key is not None
        self.streams[eng].append(o)
        return o

    def dma(self, q, out, in_, reads=(), writes=(), key=None, **kw):
        if key is None:
            key = writes[0] if writes else reads[0]
        eng_attr = {"sp": "sync", "act": "scalar", "pool": "gpsimd"}[q]

        def fn(e):
            return getattr(self.nc, eng_attr).dma_start(out=out, in_=in_, **kw)

        return self.op(q, fn, reads, writes, dma=True, key=("dma", key))

    def cc(self, kind, alu, in_ap, out_ap, reads, writes, key):
        nc = self.nc

        def fn(e):
            return nc.gpsimd.collective_compute(kind, alu, replica_groups=[list(range(8))], ins=[in_ap.opt()], outs=[out_ap.opt()])

        o = self.op("pool", fn, reads, writes, dma=True, key=("cc", key))
        o.inc = 1
        return o

    def region_begin(self, flag_ap, reads):
        for eng in ENGS:
            o = self.op(eng, None, reads, [])
            o.kind = "rb"
            o.flag = flag_ap

    def region_end(self):
        for eng in ENGS:
            o = self.op(eng, None, [], [])
            o.kind = "re"

    def finish_wait_all(self, eng="sp"):
        self._final_eng = eng

    def emit(self):
        nc = self.nc
        st = self.stack
        esem = {}
        for e in ("pe", "act", "dve", "pool"):
            esem[e] = st.enter_context(nc.semaphore("s_" + e))
        dsem = {}
        dcount = {}
        for e in ENGS:
            c = 0
            for o in self.streams[e]:
                if o.dma:
                    if o.key not in dsem:
                        dsem[o.key] = st.enter_context(nc.semaphore("d%d" % len(dsem)))
                        dcount[o.key] = 0
                    dcount[o.key] += o.inc
                    o.count = dcount[o.key]
                elif o.needs_inc:
                    c += 1
                    o.count = c
        self.n_sems = len(dsem) + 4

        def semof(o):
            return dsem[o.key] if o.dma else esem[o.eng]

        final_eng = getattr(self, "_final_eng", "sp")

        def run_stream(ename, e):
            known = {}
            region = None
            nreg = [0]
            for o in self.streams[ename]:
                waits = {}
                for d in o.deps:
                    s = semof(d)
                    k = id(s)
                    if d.count > known.get(k, 0):
                        if k not in waits or waits[k][1] < d.count:
                            waits[k] = (s, d.count)
                for k, (s, v) in waits.items():
                    e.wait_ge(s, v)
                    known[k] = v
                if o.kind == "rb":
                    nreg[0] += 1
                    rctx = e.register("rg_%s_%d" % (ename, nreg[0]))
                    rg = rctx.__enter__()
                    e.reg_load(rg, o.flag)
                    guard = e.If_ne(rg, 0)
                    guard.__enter__()
                    region = [guard, dict(known), {}, rctx]
                    continue
                if o.kind == "re":
                    region[0].__exit__(None, None, None)
                    if region[2]:
                        with e.Else():
                            for s_, n_ in region[2].values():
                                e.sem_inc(s_, n_)
                    region[3].__exit__(None, None, None)
                    known = region[1]
                    region = None
                    continue
                ins = o.fn(e)
                if o.needs_inc:
                    if o.dma:
                        ins.then_inc(dsem[o.key], o.inc)
                        s_, n_ = dsem[o.key], o.inc
                    else:
                        ins.then_inc(esem[o.eng], 1)
                        s_, n_ = esem[o.eng], 1
                    if region is not None:
                        prev = region[2].get(id(s_), (s_, 0))
                        region[2][id(s_)] = (s_, prev[1] + n_)
            if ename == final_eng:
                for key, s in dsem.items():
                    if dcount[key] > known.get(id(s), 0):
                        e.wait_ge(s, dcount[key])

        with nc.Block() as block:
            @block.sync
            def _(e):
                run_stream("sp", e)

            @block.scalar
            def _(e):
                run_stream("act", e)

            @block.vector
            def _(e):
                run_stream("dve", e)

            @block.gpsimd
            def _(e):
                run_stream("pool", e)

            @block.tensor
            def _(e):
                run_stream("pe", e)
        st.close()
import numpy as np
import ml_dtypes
from concourse.bass_utils import run_bass_kernel_spmd

NCORES = 8
D = 2048
SEQ = 8192
CTX = 256
NTOK = SEQ + CTX
TLAT = SEQ // NCORES
TL = TLAT + CTX
NT = TL // 128
KC = D // 128
EPS = 1e-6
TBLK = [(0, 512), (512, 512), (1024, 256)]


def mm(p, out, lhsT, rhs, start, stop, reads, writes):
    p.op("pe", lambda e: p.nc.tensor.matmul(out, lhsT=lhsT, rhs=rhs, start=start, stop=stop), reads, writes)


def tr(p, out, in_, ident, reads, writes):
    p.op("pe", lambda e: p.nc.tensor.transpose(out, in_, ident), list(reads) + ["ident"], writes)


def act(p, out, in_, func, reads, writes, scale=1.0, bias=0.0, accum_out=None):
    def fn(e):
        kw = {}
        if accum_out is not None:
            kw["accum_out"] = accum_out
        return p.nc.scalar.activation(out=out, in_=in_, func=func, scale=scale, bias=bias, **kw)
    p.op("act", fn, reads, writes)


def _veng(p, eng):
    return p.nc.vector if eng == "dve" else p.nc.gpsimd


def tt(p, eng, out, in0, in1, op, reads, writes):
    eng = "dve"
    p.op(eng, lambda e: _veng(p, eng).tensor_tensor(out=out, in0=in0, in1=in1, op=op), reads, writes)


def ts(p, eng, out, in0, s1, op0, reads, writes, s2=None, op1=None):
    eng = "dve"

    def fn(e):
        if op1 is None:
            return _veng(p, eng).tensor_scalar(out=out, in0=in0, scalar1=s1, scalar2=None, op0=op0)
        return _veng(p, eng).tensor_scalar(out=out, in0=in0, scalar1=s1, scalar2=s2, op0=op0, op1=op1)
    p.op(eng, fn, reads, writes)


def stt(p, eng, out, in0, scalar, in1, op0, op1, reads, writes):
    assert eng == "dve"
    p.op(eng, lambda e: _veng(p, eng).scalar_tensor_tensor(out=out, in0=in0, scalar=scalar, in1=in1, op0=op0, op1=op1), reads, writes)


def cp(p, eng, out, in_, reads, writes):
    if eng == "act":
        p.op("act", lambda e: p.nc.scalar.copy(out=out, in_=in_), reads, writes)
    else:
        eng = "dve"
        p.op(eng, lambda e: _veng(p, eng).tensor_copy(out=out, in_=in_), reads, writes)


def recip(p, out, in_, reads, writes):
    p.op("dve", lambda e: p.nc.vector.reciprocal(out=out, in_=in_), reads, writes)


def memset(p, eng, ap, val, writes):
    eng = "dve"
    p.op(eng, lambda e: _veng(p, eng).memset(ap, val), [], writes)


def make_ident(p):
    nc = p.nc
    io = p.sbuf("io_id", [128, 128])
    pc = p.sbuf("pc_id", [128, 1])
    ident = p.sbuf("ident", [128, 128])
    p.op("pool", lambda e: nc.gpsimd.iota(io[:], pattern=[[1, 128]], base=0, channel_multiplier=0, allow_small_or_imprecise_dtypes=True), [], ["io_id"])
    p.op("pool", lambda e: nc.gpsimd.iota(pc[:], pattern=[[0, 1]], base=0, channel_multiplier=1, allow_small_or_imprecise_dtypes=True), [], ["pc_id"])
    ts(p, "dve", ident[:], io[:], pc[:, 0:1], ALU.is_equal, ["io_id", "pc_id"], ["ident"])
    return ident, io, pc


def dram_in(nc, name, shape, dt=F32):
    return nc.dram_tensor(name, list(shape), dt, kind="ExternalInput").ap()


def dram_out(nc, name, shape, dt=F32):
    return nc.dram_tensor(name, list(shape), dt, kind="ExternalOutput").ap()


def adaln_T(p, P, c2T, wmod, bmodT, nchunk, wts=None, wkey="wmt"):
    nc = p.nc
    c2s = p.sbuf("c2s", [128, 32])
    bm = p.sbuf("bm", [128, nchunk])
    modT = p.sbuf("modT", [128, nchunk, 2])
    p.dma("pool", c2s[:], c2T, writes=["c2s"])
    p.dma("pool", bm[:], bmodT, writes=["bm"])
    act(p, c2s[:], c2s[:], AF.Silu, ["c2s"], ["c2s"])
    if wts is None:
        wts = [p.sbuf("wmt%d" % i, [128, KC, 256]) for i in range(2)]
    modps = P[7]
    ntile = nchunk // 2
    for t in range(ntile):
        s = t % 2
        p.dma("sp", wts[s][:], wmod[:, t * 256:(t + 1) * 256].rearrange("(k p) c -> p k c", p=128), writes=[(wkey, s)])
        for j in range(2):
            cc = t * 2 + j
            for k in range(KC):
                mm(p, modps[:, cc * 2:cc * 2 + 2], wts[s][:, k, j * 128:(j + 1) * 128], c2s[:, 2 * k:2 * k + 2],
                   k == 0, k == KC - 1, [(wkey, s), "c2s"], [("ps", 7)])
    for r in range(2):
        tt(p, "dve", modT[:, :, r], modps[:, r:2 * nchunk:2], bm[:], ALU.add, [("ps", 7), "bm"], ["modT"])
    return modT


def build_pre(nc):
    p = Prog(nc)
    x = dram_in(nc, "x", [TL, D])
    c2T = dram_in(nc, "c2T", [128, 32])
    wmod = dram_in(nc, "wmod", [D, 4096])
    bmodT = dram_in(nc, "bmodT", [128, 32])
    g1nT = dram_in(nc, "g1nT", [128, KC])
    w_in = dram_in(nc, "w_in", [D, D])
    gv = dram_in(nc, "gv", [1, 512])
    sguw = dram_in(nc, "sguw", [4, 128, 128])
    sgub = dram_in(nc, "sgub", [1, 512])
    hT_o = dram_out(nc, "hT", [128, KC, TL], BF16)
    zT_o = dram_out(nc, "zT", [1024, TL])
    ysgu_o = dram_out(nc, "ysgu", [512, TL])

    P = [p.psum("ps%d" % i, [128, 512]) for i in range(8)]
    ident, io, pc = make_ident(p)

    modT = adaln_T(p, P, c2T, wmod, bmodT, 32)
    g1n = p.sbuf("g1n", [128, KC])
    p.dma("pool", g1n[:], g1nT, writes=["g1n"])
    A1 = p.sbuf("A1", [128, 2, KC])
    for r in range(2):
        stt(p, "dve", A1[:, r, :], modT[:, 16:32, r], 1.0, g1n[:], ALU.add, ALU.mult, ["modT", "g1n"], ["A1"])

    wsq = p.sbuf("wsq", [128, 4, 128])
    WsT = p.sbuf("WsT", [128, 4, 128])
    bsb = p.sbuf("bsb", [128, 512])
    gvb = p.sbuf("gvb", [128, 512])
    p.dma("pool", wsq[:], sguw.rearrange("h q k -> q h k"), writes=["wsq"])
    p.dma("pool", bsb[:], sgub.partition_broadcast(128), writes=["bsb"])
    p.dma("pool", gvb[:], gv.partition_broadcast(128), writes=["gvb"])
    for hh in range(4):
        tr(p, P[6][:, hh * 128:(hh + 1) * 128], wsq[:, hh, :], ident[:], ["wsq"], [("ps", 6)])
    cp(p, "dve", WsT[:].rearrange("p h q -> p (h q)"), P[6][:], [("ps", 6)], ["WsT"])

    hT = p.sbuf("hTs", [128, KC, TL], BF16)
    xb = [p.sbuf("xb%d" % i, [128, D]) for i in range(2)]
    junk = p.sbuf("junk", [128, D])
    ssq = p.sbuf("ssq", [128, NT])
    rs = p.sbuf("rs", [128, NT])
    ev = 0
    for t in range(NT):
        r = 0 if t < 8 else 1
        s = t % 2
        xt = xb[s]
        p.dma("sp", xt[:], x[t * 128:(t + 1) * 128, :], writes=[("xb", s)])
        act(p, junk[:], xt[:], AF.Square, [("xb", s)], ["junk", ("ssq", t)], accum_out=ssq[:, t:t + 1])
        act(p, rs[:, t:t + 1], ssq[:, t:t + 1], AF.Sqrt, [("ssq", t)], [("rs", t)], scale=1.0 / D, bias=EPS)
        recip(p, rs[:, t:t + 1], rs[:, t:t + 1], [("rs", t)], [("rs", t)])
        ts(p, "pool", xt[:], xt[:], rs[:, t:t + 1], ALU.mult, [("xb", s), ("rs", t)], [("xb", s)])
        for kq in range(4):
            b = (t * 4 + kq) % 4
            for j in range(4):
                k = kq * 4 + j
                tr(p, P[b][:, j * 128:(j + 1) * 128], xt[:, k * 128:(k + 1) * 128], ident[:], [("xb", s)], [("ps", b)])
            for j in range(4):
                k = kq * 4 + j
                o = hT[:, k, t * 128:(t + 1) * 128]
                i_ = P[b][:, j * 128:(j + 1) * 128]
                if ev % 2 == 0:
                    ts(p, "dve", o, i_, A1[:, r, k:k + 1], ALU.mult, [("ps", b), "A1", "modT"], [("hT", t)],
                       s2=modT[:, k, r:r + 1], op1=ALU.add)
                else:
                    act(p, o, i_, AF.Identity, [("ps", b), "A1", "modT"], [("hT", t)],
                        scale=A1[:, r, k:k + 1], bias=modT[:, k, r:r + 1])
                ev += 1
    for k in range(0, KC, 4):
        p.dma("pool", hT_o[:, k:k + 4, :], hT[:, k:k + 4, :], reads=[("hT", t) for t in range(NT)], key=("hTo", k))

    wst = [p.sbuf("wst%d" % i, [128, KC, 256]) for i in range(2)]
    winbf = p.sbuf("winbf", [128, KC, 512], BF16)
    usgu = p.sbuf("usgu", [128, 4, TL])
    zst = [p.sbuf("zst%d" % i, [128, TL]) for i in range(2)]
    vt = p.sbuf("vt", [128, 512])
    vn = p.sbuf("vn", [128, 512])
    ssv = p.sbuf("ssv", [128, NT])
    rsv = p.sbuf("rsv", [128, NT])
    tmp = p.sbuf("tmp", [128, 128])
    allh = [("hT", t) for t in range(NT)]
    nz = 0
    for cb in range(4):
        for hf in range(2):
            s = hf
            p.dma("sp", wst[s][:], w_in[:, cb * 512 + hf * 256: cb * 512 + (hf + 1) * 256].rearrange("(k p) c -> p k c", p=128),
                  writes=[("wst", s)])
            cp(p, "pool" if hf else "dve", winbf[:, :, hf * 256:(hf + 1) * 256], wst[s][:], [("wst", s)], ["winbf"])
        if cb < 3:
            for c4 in range(4):
                zs = zst[nz % 2]
                for bi, (t0, tn) in enumerate(TBLK):
                    b = (nz * 3 + bi) % 4
                    for k in range(KC):
                        mm(p, P[b][:, 0:tn], winbf[:, k, c4 * 128:(c4 + 1) * 128], hT[:, k, t0:t0 + tn], k == 0, k == KC - 1,
                           ["winbf"] + allh, [("ps", b)])
                    if cb < 2:
                        cp(p, "act" if bi % 2 else "dve", zs[:, t0:t0 + tn], P[b][:, 0:tn], [("ps", b)], [("zst", nz % 2)])
                    else:
                        act(p, usgu[:, c4, t0:t0 + tn], P[b][:, 0:tn], AF.Gelu_apprx_tanh, [("ps", b)],
                            [("usgu", c4, t) for t in range(t0 // 128, (t0 + tn) // 128)])
                if cb < 2:
                    row = (cb * 4 + c4) * 128
                    p.dma("pool", zT_o[row:row + 128, :], zs[:], reads=[("zst", nz % 2)], key=("zo", nz % 2))
                nz += 1
        else:
            for t in range(NT):
                b = 4 + t % 2
                for k in range(KC):
                    mm(p, P[b][:], hT[:, k, t * 128:(t + 1) * 128], winbf[:, k, :], k == 0, k == KC - 1, ["winbf", ("hT", t)], [("ps", b)])
                act(p, vt[:], P[b][:], AF.Gelu_apprx_tanh, [("ps", b)], ["vt"])
                act(p, junk[:, 0:512], vt[:], AF.Square, ["vt"], ["junk", ("ssv", t)], accum_out=ssv[:, t:t + 1])
                act(p, rsv[:, t:t + 1], ssv[:, t:t + 1], AF.Sqrt, [("ssv", t)], [("rsv", t)], scale=1.0 / 512, bias=EPS)
                recip(p, rsv[:, t:t + 1], rsv[:, t:t + 1], [("rsv", t)], [("rsv", t)])
                stt(p, "dve", vn[:], vt[:], rsv[:, t:t + 1], gvb[:], ALU.mult, ALU.mult, ["vt", ("rsv", t), "gvb"], ["vn"])
                for hh in range(4):
                    mm(p, P[6][:, hh * 128:(hh + 1) * 128], vn[:, hh * 128:(hh + 1) * 128], WsT[:, hh, :], True, True, ["vn", "WsT"], [("ps", 6)])
                for hh in range(4):
                    tt(p, "dve", tmp[:], P[6][:, hh * 128:(hh + 1) * 128], bsb[:, hh * 128:(hh + 1) * 128], ALU.add, [("ps", 6), "bsb"], ["tmp"])
                    u_ = usgu[:, hh, t * 128:(t + 1) * 128]
                    tt(p, "pool", u_, u_, tmp[:], ALU.mult, ["tmp", ("usgu", hh, t)], [("usgu", hh, t)])
    for hh in range(4):
        p.dma("pool", ysgu_o[hh * 128:(hh + 1) * 128, :], usgu[:, hh, :], reads=[("usgu", hh, t) for t in range(NT)], key=("yso", hh))
    p.emit()
    return p


NBLK = [(i * 512, min(512, NTOK - i * 512)) for i in range((NTOK + 511) // 512)]
TWO_PI = 6.283185307179586


def frac_sincos(p, eng, x, xi, sin_out, cos_out, key, n_reads):
    cp(p, eng, xi, x, n_reads + [key + "x"], [key + "xi"])
    tt(p, eng, x, x, xi, ALU.subtract, [key + "x", key + "xi"], [key + "x"])
    if sin_out is not None:
        act(p, sin_out, x, AF.Sin, [key + "x"], [key + "sin"], scale=TWO_PI)
    if cos_out is not None:
        act(p, x, x, AF.Abs, [key + "x", key + "sin"], [key + "x"])
        act(p, cos_out, x, AF.Sin, [key + "x"], [key + "cos"], scale=-TWO_PI, bias=p.halfpi[:, 0:1])


def build_mix(nc):
    p = Prog(nc)
    uf = dram_in(nc, "uf", [64, NTOK])
    ub = dram_in(nc, "ub", [64, NTOK])
    spar = dram_in(nc, "spar", [128, 4, 3])
    bre_d = dram_in(nc, "bre", [128, 4, 32])
    bim_d = dram_in(nc, "bim", [128, 4, 32])
    cre_d = dram_in(nc, "cre", [128, 4, 64])
    cim_d = dram_in(nc, "cim", [128, 4, 64])
    zfl = dram_in(nc, "zfl", [128, SEQ])
    zfc = dram_in(nc, "zfc", [128, CTX])
    kbv_d = dram_in(nc, "kbv", [128, 8])
    ysf_o = dram_out(nc, "ysf", [64, NTOK])
    ysb_o = dram_out(nc, "ysb", [64, NTOK])
    yf_o = dram_out(nc, "yf", [128, 4096])
    yfc_o = dram_out(nc, "yfc", [128, CTX])

    P = [p.psum("ps%d" % i, [128, 512]) for i in range(8)]
    ident, io, pc = make_ident(p)
    p.halfpi = p.sbuf("halfpi", [128, 1])
    memset(p, "dve", p.halfpi[:], TWO_PI / 4, ["halfpi"])
    jio = p.sbuf("jio", [128, 512])
    p.op("pool", lambda e: nc.gpsimd.iota(jio[:], pattern=[[1, 512]], base=0, channel_multiplier=0, allow_small_or_imprecise_dtypes=True), [], ["jio"])

    sp_ = p.sbuf("spar", [128, 4, 3])
    p.dma("pool", sp_[:], spar, writes=["spar"])
    bre = p.sbuf("bre", [128, 4, 32]); bim = p.sbuf("bim", [128, 4, 32])
    cre = p.sbuf("cre", [128, 4, 64]); ncim = p.sbuf("ncim", [128, 4, 64])
    p.dma("pool", bre[:], bre_d, writes=["bre"]); p.dma("pool", bim[:], bim_d, writes=["bim"])
    p.dma("pool", cre[:], cre_d, writes=["cre"]); p.dma("pool", ncim[:], cim_d, writes=["ncim"])
    ts(p, "dve", ncim[:], ncim[:], -1.0, ALU.mult, ["ncim"], ["ncim"])
    sm = p.sbuf("sm", [128, 16, 4])
    SM = {n: sm[:, i, :] for i, n in enumerate(["dt", "ar", "th", "r", "x", "cs", "sn", "abr", "abi", "den", "cr", "ci", "t1", "t2", "nr", "x2"])}
    smi = p.sbuf("smi", [128, 4], I32)
    are, aim, ldt = sp_[:, :, 0], sp_[:, :, 1], sp_[:, :, 2]
    K_ = ["spar", "sm"]
    act(p, SM["dt"], ldt, AF.Exp, ["spar"], ["sm"])
    tt(p, "dve", SM["ar"], are, SM["dt"], ALU.mult, K_, ["sm"])
    tt(p, "dve", SM["th"], aim, SM["dt"], ALU.mult, K_, ["sm"])
    ts(p, "dve", SM["th"], SM["th"], 1.0 / TWO_PI, ALU.mult, K_, ["sm"])
    act(p, SM["r"], SM["ar"], AF.Exp, ["sm"], ["sm"])
    cp(p, "dve", SM["x"], SM["th"], K_, ["sm"])
    cp(p, "dve", smi[:], SM["x"], K_, ["smi"])
    tt(p, "dve", SM["x"], SM["x"], smi[:], ALU.subtract, ["sm", "smi"], ["sm"])
    act(p, SM["sn"], SM["x"], AF.Sin, ["sm"], ["sm"], scale=TWO_PI)
    act(p, SM["x2"], SM["x"], AF.Abs, ["sm"], ["sm"])
    act(p, SM["cs"], SM["x2"], AF.Sin, ["sm", "halfpi"], ["sm"], scale=-TWO_PI, bias=p.halfpi[:, 0:1])
    tt(p, "dve", SM["abr"], SM["r"], SM["cs"], ALU.mult, ["sm"], ["sm"])
    tt(p, "dve", SM["abi"], SM["r"], SM["sn"], ALU.mult, ["sm"], ["sm"])
    ts(p, "dve", SM["nr"], SM["abr"], -1.0, ALU.add, ["sm"], ["sm"])
    tt(p, "dve", SM["den"], are, are, ALU.mult, K_, ["sm"])
    tt(p, "dve", SM["t1"], aim, aim, ALU.mult, K_, ["sm"])
    tt(p, "dve", SM["den"], SM["den"], SM["t1"], ALU.add, ["sm"], ["sm"])
    recip(p, SM["den"], SM["den"], ["sm"], ["sm"])
    tt(p, "dve", SM["t1"], SM["nr"], are, ALU.mult, K_, ["sm"])
    tt(p, "dve", SM["t2"], SM["abi"], aim, ALU.mult, K_, ["sm"])
    tt(p, "dve", SM["cr"], SM["t1"], SM["t2"], ALU.add, ["sm"], ["sm"])
    tt(p, "dve", SM["cr"], SM["cr"], SM["den"], ALU.mult, ["sm"], ["sm"])
    tt(p, "dve", SM["t1"], SM["abi"], are, ALU.mult, K_, ["sm"])
    tt(p, "dve", SM["t2"], SM["nr"], aim, ALU.mult, K_, ["sm"])
    tt(p, "dve", SM["ci"], SM["t1"], SM["t2"], ALU.subtract, ["sm"], ["sm"])
    tt(p, "dve", SM["ci"], SM["ci"], SM["den"], ALU.mult, ["sm"], ["sm"])
    bbr = p.sbuf("bbr", [128, 4, 32]); bbi = p.sbuf("bbi", [128, 4, 32]); btmp = p.sbuf("btmp", [128, 32])
    lhsB = p.sbuf("lhsB", [32, 4, 2, 128])
    for ti in range(4):
        crc, cic = SM["cr"][:, ti:ti + 1], SM["ci"][:, ti:ti + 1]
        ts(p, "dve", btmp[:], bim[:, ti, :], cic, ALU.mult, ["bim", "sm"], ["btmp"])
        stt(p, "dve", bbr[:, ti, :], bre[:, ti, :], crc, btmp[:], ALU.mult, ALU.subtract, ["bre", "sm", "btmp"], ["bbr"])
        ts(p, "dve", btmp[:], bre[:, ti, :], cic, ALU.mult, ["bre", "sm", "bbr"], ["btmp"])
        stt(p, "dve", bbi[:, ti, :], bim[:, ti, :], crc, btmp[:], ALU.mult, ALU.add, ["bim", "sm", "btmp"], ["bbi"])
    for ti in range(4):
        bk = 6 + ti // 2
        for ri, src in enumerate((bbr, bbi)):
            c0 = ((ti % 2) * 2 + ri) * 128
            tr(p, P[bk][0:32, c0:c0 + 128], src[:, ti, :], ident[:], ["bbr", "bbi"], [("ps", bk)])
    for hlf in range(2):
        cp(p, "dve", lhsB[:, 2 * hlf:2 * hlf + 2, :, :].rearrange("p a b c -> p (a b c)"), P[6 + hlf][0:32, :], [("ps", 6 + hlf)], ["lhsB"])
    tabc = p.sbuf("tabc", [128, 4, 512]); tabs = p.sbuf("tabs", [128, 4, 512])
    tx = p.sbuf("tx", [128, 512]); txi = p.sbuf("txi", [128, 512], I32)
    for ti in range(4):
        ts(p, "dve", tx[:], jio[:], SM["th"][:, ti:ti + 1], ALU.mult, ["jio", "sm", "Tcos", "Tsin"], ["Tx"])
        frac_sincos(p, "dve", tx[:], txi[:], tabs[:, ti, :], tabc[:, ti, :], "T", [])

    NW = 2
    ubuf = [[p.sbuf("u%d_%d" % (ti, i), [32, 512]) for i in range(NW)] for ti in range(4)]
    W = {}
    for nm in ["bre", "bim", "t1", "t2", "inre", "inim", "wre", "wim", "sre", "sim"]:
        W[nm] = [p.sbuf("w_%s%d" % (nm, i), [128, 512]) for i in range(2)]
    carry = p.sbuf("carry", [128, 4, 2])
    ctmp = p.sbuf("ctmp", [128, 4, 2])
    memset(p, "dve", carry[:], 0.0, ["carry"])
    yst = [[p.sbuf("yst%d_%d" % (d, i), [64, 512]) for i in range(2)] for d in range(2)]
    usrc = [uf, ub]
    it = 0
    for bi, (t0, tn) in enumerate(NBLK):
        for d in range(2):
            for gp in range(2):
                ti = d * 2 + gp
                ws = it % 2
                us = bi % NW
                u = ubuf[ti][us]
                p.dma("sp", u[:, 0:tn], usrc[d][gp * 32:(gp + 1) * 32, t0:t0 + tn], writes=[("u", ti, us)])
                b0, b1 = (0, 1) if it % 2 == 0 else (2, 3)
                mm(p, P[b0][:, 0:tn], lhsB[:, ti, 0, :], u[:, 0:tn], True, True, ["lhsB", ("u", ti, us)], [("ps", b0)])
                mm(p, P[b1][:, 0:tn], lhsB[:, ti, 1, :], u[:, 0:tn], True, True, ["lhsB", ("u", ti, us)], [("ps", b1)])
                k = lambda nm: ("w", nm, ws)
                a = lambda nm: W[nm][ws][:, 0:tn]
                cp(p, "act", a("bre"), P[b0][:, 0:tn], [("ps", b0)], [k("bre")])
                cp(p, "act", a("bim"), P[b1][:, 0:tn], [("ps", b1)], [k("bim")])
                cs_, sn_ = tabc[:, ti, 0:tn], tabs[:, ti, 0:tn]
                tt(p, "pool", a("t1"), a("bre"), cs_, ALU.mult, [k("bre"), "Tcos"], [k("t1")])
                tt(p, "pool", a("t2"), a("bim"), sn_, ALU.mult, [k("bim"), "Tsin"], [k("t2")])
                tt(p, "pool", a("inre"), a("t1"), a("t2"), ALU.add, [k("t1"), k("t2")], [k("inre")])
                tt(p, "pool", a("t1"), a("bim"), cs_, ALU.mult, [k("bim"), "Tcos"], [k("t1")])
                tt(p, "pool", a("t2"), a("bre"), sn_, ALU.mult, [k("bre"), "Tsin"], [k("t2")])
                tt(p, "pool", a("inim"), a("t1"), a("t2"), ALU.subtract, [k("t1"), k("t2")], [k("inim")])
                rr = SM["r"][:, ti:ti + 1]
                p.op("dve", lambda e, o=a("wre"), i=a("inre"), c=carry[:, ti, 0:1], rr=rr, tn=tn: nc.vector.tensor_tensor_scan(
                    out=o, data0=rr.to_broadcast([128, tn]), data1=i, initial=c, op0=ALU.mult, op1=ALU.add),
                    [k("inre"), ("carry", ti), "sm"], [k("wre")])
                p.op("dve", lambda e, o=a("wim"), i=a("inim"), c=carry[:, ti, 1:2], rr=rr, tn=tn: nc.vector.tensor_tensor_scan(
                    out=o, data0=rr.to_broadcast([128, tn]), data1=i, initial=c, op0=ALU.mult, op1=ALU.add),
                    [k("inim"), ("carry", ti), "sm"], [k("wim")])
                tt(p, "dve", a("t1"), a("wre"), cs_, ALU.mult, [k("wre"), "Tcos", k("inre"), k("inim")], [k("t1")])
                tt(p, "dve", a("t2"), a("wim"), sn_, ALU.mult, [k("wim"), "Tsin", k("inre"), k("inim")], [k("t2")])
                tt(p, "dve", a("sre"), a("t1"), a("t2"), ALU.subtract, [k("t1"), k("t2")], [k("sre")])
                tt(p, "pool", a("inre"), a("wre"), sn_, ALU.mult, [k("wre"), "Tsin"], [k("inre")])
                tt(p, "pool", a("inim"), a("wim"), cs_, ALU.mult, [k("wim"), "Tcos"], [k("inim")])
                tt(p, "pool", a("sim"), a("inre"), a("inim"), ALU.add, [k("inre"), k("inim")], [k("sim")])
                sl_re, sl_im = W["sre"][ws][:, tn - 1:tn], W["sim"][ws][:, tn - 1:tn]
                csc, snc = SM["cs"][:, ti:ti + 1], SM["sn"][:, ti:ti + 1]
                ts(p, "dve", ctmp[:, ti, 0:1], sl_im, snc, ALU.mult, [k("sim"), "sm"], [("ctmp", ti)])
                stt(p, "dve", carry[:, ti, 0:1], sl_re, csc, ctmp[:, ti, 0:1], ALU.mult, ALU.subtract, [k("sre"), ("ctmp", ti), "sm"], [("carry", ti)])
                ts(p, "dve", ctmp[:, ti, 1:2], sl_re, snc, ALU.mult, [k("sre"), "sm"], [("ctmp", ti)])
                stt(p, "dve", carry[:, ti, 1:2], sl_im, csc, ctmp[:, ti, 1:2], ALU.mult, ALU.add, [k("sim"), ("ctmp", ti), "sm"], [("carry", ti)])
                yb = 4 + d
                mm(p, P[yb][0:64, 0:tn], cre[:, ti, :], a("sre"), gp == 0, False, ["cre", k("sre")], [("ps", yb)])
                mm(p, P[yb][0:64, 0:tn], ncim[:, ti, :], a("sim"), False, gp == 1, ["ncim", k("sim")], [("ps", yb)])
                it += 1
            ys = yst[d][bi % 2]
            cp(p, "act", ys[:, 0:tn], P[4 + d][0:64, 0:tn], [("ps", 4 + d)], [("yst", d, bi % 2)])
            p.dma("pool", (ysf_o if d == 0 else ysb_o)[:, t0:t0 + tn], ys[:, 0:tn], reads=[("yst", d, bi % 2)], key=("yso", d, bi % 2))

    kbv = p.sbuf("kbv", [128, 8])
    p.dma("pool", kbv[:], kbv_d, writes=["kbv"])
    cs128 = p.sbuf("cs128", [128, 256])
    fx = p.sbuf("fx", [128, 512]); fxi = p.sbuf("fxi", [128, 512], I32)
    ts(p, "dve", fx[:, 0:128], io[:], pc[:, 0:1], ALU.mult, ["io_id", "pc_id"], ["Fx"], s2=1.0 / 128, op1=ALU.mult)
    frac_sincos(p, "dve", fx[:, 0:128], fxi[:, 0:128], cs128[:, 128:256], cs128[:, 0:128], "F", [])
    nval = p.sbuf("nval", [128, 64])
    p.op("pool", lambda e: nc.gpsimd.iota(nval[:], pattern=[[128, 64]], base=0, channel_multiplier=1, allow_small_or_imprecise_dtypes=True), [], ["nval"])
    bx = p.sbuf("bx", [128, 64, 8]); bxi = p.sbuf("bxi", [128, 512], I32)
    cbt = p.sbuf("cbt", [128, 64, 8]); sbt = p.sbuf("sbt", [128, 64, 8]); ncbt = p.sbuf("ncbt", [128, 64, 8])
    for kb in range(8):
        ts(p, "dve", bx[:, :, kb], nval[:], kbv[:, kb:kb + 1], ALU.mult, ["nval", "kbv"], ["Bx"], s2=1.0 / 16, op1=ALU.mult)
    bxf = bx[:].rearrange("p a b -> p (a b)")
    frac_sincos(p, "dve", bxf, bxi[:], sbt[:].rearrange("p a b -> p (a b)"), cbt[:].rearrange("p a b -> p (a b)"), "B", [])
    ts(p, "dve", ncbt[:].rearrange("p a b -> p (a b)"), cbt[:].rearrange("p a b -> p (a b)"), -1.0, ALU.mult, ["Bcos"], ["ncbt"])

    def fnet(zsrc, N, ntile, nkb, kw, out_ap, scale, tagname):
        CH = min(N, 2048)
        zsb = [p.sbuf("zs%d_%s" % (i, tagname), [128, CH]) for i in range(2 if N > CH else 1)]
        AB = p.sbuf("AB_" + tagname, [128, ntile, 256], BF16 if ntile > 2 else F32)
        for nt in range(ntile):
            b = nt % 2
            ci, off = divmod(nt * 128, CH)
            zs = zsb[ci % 2]
            if off == 0:
                p.dma("sp", zs[:], zsrc[:, ci * CH:(ci + 1) * CH], writes=[("zs" + tagname, ci % 2)])
            mm(p, P[b][:, 0:256], zs[:, off:off + 128], cs128[:], True, True, [("zs" + tagname, ci % 2), "Fcos", "Fsin"], [("ps", b)])
            cp(p, "act" if nt % 2 else "dve", AB[:, nt, :], P[b][:, 0:256], [("ps", b)], [("AB" + tagname, nt)])
        Ca = [p.sbuf("Ca%d_%s" % (i, tagname), [128, kw], BF16) for i in range(2)]
        Sa = [p.sbuf("Sa%d_%s" % (i, tagname), [128, kw], BF16) for i in range(2)]
        PQ = [p.sbuf("PQ%d_%s" % (i, tagname), [128, 2, 128], BF16) for i in range(4)]
        qt = [p.sbuf("qt%d_%s" % (i, tagname), [128, 2, 128]) for i in range(2)]
        cx = p.sbuf("cx_" + tagname, [128, kw]); cxi = p.sbuf("cxi_" + tagname, [128, kw], I32)
        npq = 0
        for nt in range(ntile):
            s = nt % 2
            tk = "C" + tagname
            ts(p, "dve", cx[:], jio[:, 0:kw], nval[:, nt:nt + 1], ALU.mult, ["jio", "nval", tk + "sin", tk + "cos"], [tk + "x"], s2=1.0 / N, op1=ALU.mult)
            cp(p, "dve", cxi[:], cx[:], [tk + "x"], [tk + "xi"])
            tt(p, "dve", cx[:], cx[:], cxi[:], ALU.subtract, [tk + "x", tk + "xi"], [tk + "x"])
            act(p, Sa[s][:], cx[:], AF.Sin, [tk + "x"], [tk + "sin", ("Sa" + tagname, s)], scale=TWO_PI)
            act(p, cx[:], cx[:], AF.Abs, [tk + "x", tk + "sin"], [tk + "x"])
            act(p, Ca[s][:], cx[:], AF.Sin, [tk + "x", "halfpi"], [tk + "cos", ("Ca" + tagname, s)], scale=-TWO_PI, bias=p.halfpi[:, 0:1])
            A_, B_ = AB[:, nt, 0:128], AB[:, nt, 128:256]
            for kb in range(nkb):
                pq = PQ[npq % 4]; q = qt[npq % 2]; eng = "dve" if npq % 2 == 0 else "pool"
                kq = ("PQ" + tagname, npq % 4); kt = ("qt" + tagname, npq % 2)
                if nkb == 1:
                    cp(p, eng, pq[:, 0, :], A_, [("AB" + tagname, nt)], [kq])
                    ts(p, eng, pq[:, 1, :], B_, -1.0, ALU.mult, [("AB" + tagname, nt)], [kq])
                else:
                    cb_, sb_, ncb_ = cbt[:, nt, kb:kb + 1], sbt[:, nt, kb:kb + 1], ncbt[:, nt, kb:kb + 1]
                    ts(p, "pool", q[:, 0, :], B_, sb_, ALU.mult, [("AB" + tagname, nt), "Bsin"], [kt])
                    ts(p, "pool", q[:, 1, :], A_, sb_, ALU.mult, [("AB" + tagname, nt), "Bsin"], [kt])
                    stt(p, "dve", pq[:, 0, :], A_, cb_, q[:, 0, :], ALU.mult, ALU.subtract, [("AB" + tagname, nt), "Bcos", kt], [kq])
                    stt(p, "dve", pq[:, 1, :], B_, ncb_, q[:, 1, :], ALU.mult, ALU.subtract, [("AB" + tagname, nt), "ncbt", kt], [kq])
                mm(p, P[kb][:, 0:kw], pq[:, 0, :], Ca[s][:], nt == 0, False, [kq, ("Ca" + tagname, s)], [("ps", kb)])
                mm(p, P[kb][:, 0:kw], pq[:, 1, :], Sa[s][:], False, nt == ntile - 1, [kq, ("Sa" + tagname, s)], [("ps", kb)])
                npq += 1
        yst_ = p.sbuf("yfst_" + tagname, [128, nkb * kw])
        for kb in range(nkb):
            act(p, yst_[:, kb * kw:(kb + 1) * kw], P[kb][:, 0:kw], AF.Copy, [("ps", kb)], ["yfst" + tagname], scale=scale)
        p.dma("pool", out_ap, yst_[:], reads=["yfst" + tagname], key="yfo" + tagname)

    fnet(zfc, CTX, 2, 1, 256, yfc_o, 1.0 / float(np.sqrt(CTX * 128.0)), "c")
    fnet(zfl, SEQ, 64, 8, 512, yf_o, 1.0 / 1024.0, "l")
    p.emit()
    return p


def build_posta1(nc):
    p = Prog(nc)
    hT_d = dram_in(nc, "hT", [128, KC, TL], BF16)
    uT_d = dram_in(nc, "uT", [512, TL])
    ysf_d = dram_in(nc, "ysf", [512, TL])
    ysb_d = dram_in(nc, "ysb", [512, TL])
    yf_d = dram_in(nc, "yf", [512, TL])
    ysgu_d = dram_in(nc, "ysgu", [512, TL])
    s5d_d = dram_in(nc, "s5d", [128, 4])
    wglu_d = dram_in(nc, "wglu", [512, 512])
    wbr_d = dram_in(nc, "wbr", [3, 512, D])
    wgate_d = dram_in(nc, "wgate", [D, 3 * D])
    bgT_d = dram_in(nc, "bgT", [128, 48])
    mT_o = dram_out(nc, "mT", [128, KC, TL], BF16)

    P = [p.psum("ps%d" % i, [128, 512]) for i in range(8)]
    hT = p.sbuf("hTs", [128, KC, TL], BF16)
    for k in range(0, KC, 4):
        p.dma("sp", hT[:, k:k + 4, :], hT_d[:, k:k + 4, :], writes=[("hT", k)])
    allh = [("hT", k) for k in range(0, KC, 4)]
    s5d = p.sbuf("s5d", [128, 4]); bgT = p.sbuf("bgT", [128, 48])
    p.dma("pool", s5d[:], s5d_d, writes=["s5d"]); p.dma("pool", bgT[:], bgT_d, writes=["bgT"])
    feats = [p.sbuf("feat%d" % i, [128, 4, TL], BF16) for i in range(3)]
    yf32 = p.sbuf("yf32", [128, 4, TL]); ybf = p.sbuf("ybf", [128, 4, TL], BF16)
    st = [p.sbuf("st%d" % i, [128, TL]) for i in range(3)]
    wb32 = p.sbuf("wb32", [128, 4, 512]); wgl = p.sbuf("wgl", [128, 4, 512], BF16)
    p.dma("pool", wb32[:], wglu_d.rearrange("(c p) n -> p c n", p=128), writes=["wb32"])
    cp(p, "pool", wgl[:], wb32[:], ["wb32"], ["wgl"])
    for c in range(4):
        rows = slice(c * 128, (c + 1) * 128)
        p.dma("sp", st[0][:], ysf_d[rows, :], writes=[("st", 0)])
        p.dma("sp", st[1][:], ysb_d[rows, :], writes=[("st", 1)])
        p.dma("sp", st[2][:], uT_d[rows, :], writes=[("st", 2)])
        tt(p, "pool", st[0][:], st[0][:], st[1][:], ALU.add, [("st", 0), ("st", 1)], [("st", 0)])
        stt(p, "dve", st[0][:], st[2][:], s5d[:, c:c + 1], st[0][:], ALU.mult, ALU.add, [("st", 0), ("st", 2), "s5d"], [("st", 0)])
        act(p, yf32[:, c, :], st[0][:], AF.Gelu_apprx_tanh, [("st", 0)], [("yf32", c)])
        cp(p, "pool", ybf[:, c, :], yf32[:, c, :], [("yf32", c)], [("ybf", c)])
    sig = p.sbuf("sig", [128, 512])
    ally = [("ybf", c) for c in range(4)]
    for c2 in range(4):
        for bi, (t0, tn) in enumerate(TBLK):
            b = (c2 * 3 + bi) % 2
            for c in range(4):
                mm(p, P[b][:, 0:tn], wgl[:, c, c2 * 128:(c2 + 1) * 128], ybf[:, c, t0:t0 + tn], c == 0, c == 3, ["wgl"] + ally, [("ps", b)])
            act(p, sig[:, 0:tn], P[b][:, 0:tn], AF.Sigmoid, [("ps", b)], ["sig"])
            tt(p, "dve", feats[0][:, c2, t0:t0 + tn], yf32[:, c2, t0:t0 + tn], sig[:, 0:tn], ALU.mult, ["sig", ("yf32", c2)], [("feat", 0)])
    for fi, src in ((1, yf_d), (2, ysgu_d)):
        for c in range(4):
            s = (fi * 4 + c) % 3
            p.dma("sp", st[s][:], src[c * 128:(c + 1) * 128, :], writes=[("st", s)])
            cp(p, "pool" if c % 2 else "dve", feats[fi][:, c, :], st[s][:], [("st", s)], [("feat", fi)])
    wst = [p.sbuf("wst%d" % i, [128, KC, 256]) for i in range(2)]
    wgb = p.sbuf("wgb", [128, KC, 512], BF16)
    wbb = p.sbuf("wbb", [128, 4, 512], BF16)
    merged = yf32
    _m = p.sbuf("mTs0", [128, 4, TL], BF16)
    mTs = [_m, _m]
    gate = [p.sbuf("gate%d" % i, [128, 512]) for i in range(2)]
    gtmp = [p.sbuf("gtmp%d" % i, [128, 512]) for i in range(2)]
    n = 0
    for dq in range(4):
        for kbr in range(3):
            col0 = kbr * D + dq * 512
            for hf in range(2):
                p.dma("sp", wst[hf][:], wgate_d[:, col0 + hf * 256: col0 + (hf + 1) * 256].rearrange("(k p) c -> p k c", p=128), writes=[("wst", hf)])
                cp(p, "pool" if hf else "dve", wgb[:, :, hf * 256:(hf + 1) * 256], wst[hf][:], [("wst", hf)], ["wgb"])
            p.dma("pool", wb32[:], wbr_d[kbr, :, dq * 512:(dq + 1) * 512].rearrange("(c p) n -> p c n", p=128), writes=["wb32"])
            cp(p, "pool", wbb[:], wb32[:], ["wb32"], ["wbb"])
            for dl in range(4):
                dc = dq * 4 + dl
                for bi, (t0, tn) in enumerate(TBLK):
                    bg = (n % 2) * 2; bb_ = bg + 1
                    for k in range(KC):
                        mm(p, P[bg][:, 0:tn], wgb[:, k, dl * 128:(dl + 1) * 128], hT[:, k, t0:t0 + tn], k == 0, k == KC - 1, ["wgb"] + allh, [("ps", bg)])
                    for c in range(4):
                        mm(p, P[bb_][:, 0:tn], wbb[:, c, dl * 128:(dl + 1) * 128], feats[kbr][:, c, t0:t0 + tn], c == 0, c == 3, ["wbb", ("feat", kbr)], [("ps", bb_)])
                    g_ = gate[n % 2]
                    act(p, g_[:, 0:tn], P[bg][:, 0:tn], AF.Sigmoid, [("ps", bg)], [("gate", n % 2)], bias=bgT[:, kbr * 16 + dc: kbr * 16 + dc + 1])
                    mk = ("merged", dl, bi)
                    if kbr == 0:
                        tt(p, "dve", merged[:, dl, t0:t0 + tn], P[bb_][:, 0:tn], g_[:, 0:tn], ALU.mult, [("ps", bb_), ("gate", n % 2)], [mk])
                    else:
                        gt = gtmp[n % 2]
                        tt(p, "dve", gt[:, 0:tn], P[bb_][:, 0:tn], g_[:, 0:tn], ALU.mult, [("ps", bb_), ("gate", n % 2)], [("gtmp", n % 2)])
                        if kbr == 1:
                            tt(p, "pool", merged[:, dl, t0:t0 + tn], merged[:, dl, t0:t0 + tn], gt[:, 0:tn], ALU.add, [("gtmp", n % 2), mk], [mk])
                        else:
                            tt(p, "pool", mTs[dq % 2][:, dl, t0:t0 + tn], merged[:, dl, t0:t0 + tn], gt[:, 0:tn], ALU.add, [("gtmp", n % 2), mk], [("mTs", 0)])
                    n += 1
        p.dma("pool", mT_o[:, dq * 4:(dq + 1) * 4, :], mTs[0][:], reads=[("mTs", 0)], key=("mTo", 0))
    p.emit()
    return p


def build_posta2(nc):
    p = Prog(nc)
    x_d = dram_in(nc, "x", [TL, D])
    mT_d = dram_in(nc, "mT", [128, KC, TL], BF16)
    c2T = dram_in(nc, "c2T", [128, 32])
    wmod = dram_in(nc, "wmod", [D, 6144])
    bmodT = dram_in(nc, "bmodT", [128, 48])
    g2nT = dram_in(nc, "g2nT", [128, KC])
    wout_d = dram_in(nc, "wout", [D, D])
    rw_d = dram_in(nc, "rw", [D, 32])
    rb_d = dram_in(nc, "rb", [1, 32])
    x1_o = dram_out(nc, "x1", [TL, D])
    h2T_o = dram_out(nc, "h2T", [128, KC, TL])
    rwt_o = dram_out(nc, "rwt", [TL, 32])

    P = [p.psum("ps%d" % i, [128, 512]) for i in range(8)]
    ident, io, pc = make_ident(p)
    wst = [p.sbuf("wst%d" % i, [128, KC, 256]) for i in range(2)]
    modT = adaln_T(p, P, c2T, wmod, bmodT, 48, wts=wst, wkey="wst")
    g2n = p.sbuf("g2n", [128, KC])
    p.dma("pool", g2n[:], g2nT, writes=["g2n"])
    A2 = p.sbuf("A2", [128, 2, KC])
    for r in range(2):
        stt(p, "dve", A2[:, r, :], modT[:, 32:48, r], 1.0, g2n[:], ALU.add, ALU.mult, ["modT", "g2n"], ["A2"])
    ones = p.sbuf("ones", [128, 128])
    memset(p, "dve", ones[:], 1.0, ["ones"])
    g1b = p.sbuf("g1b", [128, 2, D])
    dg = [p.sbuf("dg%d" % i, [128, 128]) for i in range(2)]
    for r in range(2):
        for dc in range(KC):
            s = dc % 2
            ts(p, "dve", dg[s][:], ident[:], modT[:, dc, r:r + 1], ALU.mult, ["ident", "modT"], [("dg", s)])
            b = (dc // 4) % 2
            mm(p, P[b][:, (dc % 4) * 128:(dc % 4 + 1) * 128], ones[:], dg[s][:], True, True, ["ones", ("dg", s)], [("ps", b)])
            if dc % 4 == 3:
                cp(p, "act", g1b[:, r, (dc // 4) * 512:(dc // 4 + 1) * 512], P[b][:], [("ps", b)], ["g1b"])
    rw = p.sbuf("rws", [128, KC, 32]); rbb = p.sbuf("rbb", [128, 32])
    p.dma("pool", rw[:], rw_d.rearrange("(k p) e -> p k e", p=128), writes=["rw"])
    p.dma("pool", rbb[:], rb_d.partition_broadcast(128), writes=["rbb"])
    mT = p.sbuf("mTs", [128, KC, TL], BF16)
    for k in range(0, KC, 4):
        p.dma("sp", mT[:, k:k + 4, :], mT_d[:, k:k + 4, :], writes=[("mT", k)])
    allm = [("mT", k) for k in range(0, KC, 4)]
    wo = p.sbuf("wo", [128, KC, D], BF16)
    for t in range(8):
        s = t % 2
        p.dma("sp", wst[s][:], wout_d[:, t * 256:(t + 1) * 256].rearrange("(k p) c -> p k c", p=128), writes=[("wst", s)])
        cp(p, "pool" if s else "dve", wo[:, :, t * 256:(t + 1) * 256], wst[s][:], [("wst", s)], [("wo", t // 2)])
    xb = [p.sbuf("xb%d" % i, [128, D]) for i in range(2)]
    x1b = p.sbuf("x1b", [128, D])
    junk = p.sbuf("junk", [128, D], BF16)
    h2t = [p.sbuf("h2t%d" % i, [128, KC, 128]) for i in range(2)]
    ssq = p.sbuf("ssq", [128, NT]); rs = p.sbuf("rs", [128, NT])
    otmp = [p.sbuf("otmp%d" % i, [128, 512]) for i in range(2)]
    lg = p.sbuf("lg", [128, 32]); top8 = p.sbuf("top8", [128, 8]); msk = p.sbuf("msk", [128, 32])
    nmx = p.sbuf("nmx", [128, 1]); ex = p.sbuf("ex", [128, 32]); se = p.sbuf("se", [128, 1]); rwo = [p.sbuf("rwo%d" % i, [128, 32]) for i in range(2)]
    ev = 0
    for t in range(NT):
        r = 0 if t < 8 else 1
        s = t % 2
        xt = xb[s]
        p.dma("sp", xt[:], x_d[t * 128:(t + 1) * 128, :], writes=[("xb", s)])
        for cb in range(4):
            b = (t * 4 + cb) % 4
            for k in range(KC):
                mm(p, P[b][:], mT[:, k, t * 128:(t + 1) * 128], wo[:, k, cb * 512:(cb + 1) * 512], k == 0, k == KC - 1, allm + [("wo", cb)], [("ps", b)])
            ot = otmp[cb % 2]
            tt(p, "dve", ot[:], P[b][:], g1b[:, r, cb * 512:(cb + 1) * 512], ALU.mult, [("ps", b), "g1b"], [("otmp", cb % 2)])
            tt(p, "pool", x1b[:, cb * 512:(cb + 1) * 512], xt[:, cb * 512:(cb + 1) * 512], ot[:], ALU.add, [("otmp", cb % 2), ("xb", s)], ["x1b"])
        p.dma("pool", x1_o[t * 128:(t + 1) * 128, :], x1b[:], reads=["x1b"], key="x1o")
        act(p, junk[:], x1b[:], AF.Square, ["x1b"], ["junk", ("ssq", t)], accum_out=ssq[:, t:t + 1])
        act(p, rs[:, t:t + 1], ssq[:, t:t + 1], AF.Sqrt, [("ssq", t)], [("rs", t)], scale=1.0 / D, bias=EPS)
        recip(p, rs[:, t:t + 1], rs[:, t:t + 1], [("rs", t)], [("rs", t)])
        ts(p, "pool", xt[:], x1b[:], rs[:, t:t + 1], ALU.mult, ["x1b", ("rs", t), ("xb", s)], [("xb", s)])
        h2 = h2t[s]
        for kq in range(4):
            b = 4 + (t * 4 + kq) % 2
            for j in range(4):
                k = kq * 4 + j
                tr(p, P[b][:, j * 128:(j + 1) * 128], xt[:, k * 128:(k + 1) * 128], ident[:], [("xb", s)], [("ps", b)])
            for j in range(4):
                k = kq * 4 + j
                i_ = P[b][:, j * 128:(j + 1) * 128]
                if ev % 2 == 0:
                    ts(p, "dve", h2[:, k, :], i_, A2[:, r, k:k + 1], ALU.mult, [("ps", b), "A2", "modT"], [("h2t", s)],
                       s2=modT[:, 16 + k, r:r + 1], op1=ALU.add)
                else:
                    act(p, h2[:, k, :], i_, AF.Identity, [("ps", b), "A2", "modT"], [("h2t", s)],
                        scale=A2[:, r, k:k + 1], bias=modT[:, 16 + k, r:r + 1])
                ev += 1
        p.dma("pool", h2T_o[:, :, t * 128:(t + 1) * 128], h2[:], reads=[("h2t", s)], key=("h2o", s))
        for k in range(KC):
            mm(p, P[6][:, 0:32], h2[:, k, :], rw[:, k, :], k == 0, k == KC - 1, [("h2t", s), "rw"], [("ps", 6)])
        tt(p, "dve", lg[:], P[6][:, 0:32], rbb[:], ALU.add, [("ps", 6), "rbb"], ["lg"])
        p.op("dve", lambda e: nc.vector.max(out=top8[:], in_=lg[:]), ["lg"], ["top8"])
        ts(p, "dve", msk[:], lg[:], top8[:, 3:4], ALU.is_ge, ["lg", "top8"], ["msk"])
        ts(p, "dve", nmx[:], top8[:, 0:1], -1.0, ALU.mult, ["top8"], ["nmx"])
        act(p, ex[:], lg[:], AF.Exp, ["lg", "nmx"], ["ex"], bias=nmx[:, 0:1])
        tt(p, "dve", ex[:], ex[:], msk[:], ALU.mult, ["ex", "msk"], ["ex"])
        p.op("dve", lambda e: nc.vector.reduce_sum(out=se[:], in_=ex[:], axis=AX.X), ["ex"], ["se"])
        recip(p, se[:], se[:], ["se"], ["se"])
        ro = rwo[s]
        ts(p, "dve", ro[:], ex[:], se[:, 0:1], ALU.mult, ["ex", "se"], [("rwo", s)])
        p.dma("pool", rwt_o[t * 128:(t + 1) * 128, :], ro[:], reads=[("rwo", s)], key=("rwo", s))
    p.emit()
    return p


CAP = 4096
SCH = 1024
NBC = SCH // 128
NB = CAP // 128
NTT = NTOK // 128
NROW = NTOK + CAP
SW_LIMIT = 7.0
SW_ALPHA = 1.702
SBLK = [(0, 512), (512, 512)]


def build_moe(nc):
    p = Prog(nc)
    h2p = dram_in(nc, "h2p", [NROW, D])
    rwT_d = dram_in(nc, "rwT", [128, NTT * 4])
    wup_d = dram_in(nc, "wup", [4, D, 2 * D])
    bupT_d = dram_in(nc, "bupT", [128, 4 * 32])
    wdn_d = dram_in(nc, "wdn", [4, D, D])
    bdn_d = dram_in(nc, "bdn", [4, D])
    yp_o = [dram_out(nc, "yp%d" % i, [NROW, 512]) for i in range(4)]

    P = [p.psum("ps%d" % i, [128, 512]) for i in range(8)]
    ident, io, pc = make_ident(p)

    zt = p.sbuf("zt", [128, 512])
    memset(p, "dve", zt[:], 0.0, ["zt"])
    nz = 0
    yz_keys = []
    for i in range(4):
        for r0 in range(0, NROW, 128 * 18):
            nr = min(128 * 18, NROW - r0)
            k_ = ("yz", nz); yz_keys.append(k_); nz += 1
            p.dma("pool", yp_o[i][r0:r0 + nr, :].rearrange("(a p) c -> p a c", p=128), zt[:].unsqueeze(1).to_broadcast([128, nr // 128, 512]),
                  reads=["zt"], writes=[k_], key="yz")

    rw = p.sbuf("rw", [128, NTT, 4]); msk = p.sbuf("msk", [128, NTT, 4]); rank = p.sbuf("rank", [128, NTT, 4])
    p.dma("sp", rw[:].rearrange("p a b -> p (a b)"), rwT_d, writes=["rw"])
    ts(p, "dve", msk[:], rw[:], 0.0, ALU.is_gt, ["rw"], ["msk"])
    ones = p.sbuf("ones", [128, 128]); Ls = p.sbuf("Ls", [128, 128])
    memset(p, "dve", ones[:], 1.0, ["ones"])
    ts(p, "dve", Ls[:], io[:], pc[:, 0:1], ALU.is_gt, ["io_id", "pc_id"], ["Ls"])
    for t in range(NTT):
        mm(p, P[0][:, t * 4:(t + 1) * 4], Ls[:], msk[:, t, :], True, True, ["Ls", "msk"], [("ps", 0)])
    mm(p, P[1][:, 0:NTT * 4], ones[:], msk[:].rearrange("p a b -> p (a b)"), True, True, ["ones", "msk"], [("ps", 1)])
    tot = p.sbuf("tot", [128, NTT, 4]); cum = p.sbuf("cum", [128, NTT, 4])
    cp(p, "dve", tot[:].rearrange("p a b -> p (a b)"), P[1][:, 0:NTT * 4], [("ps", 1)], ["tot"])
    for e in range(4):
        p.op("dve", lambda e_, e=e: nc.vector.tensor_tensor_scan(out=cum[:, :, e], data0=ones[:, 0:NTT], data1=tot[:, :, e], initial=0.0,
                                                                 op0=ALU.mult, op1=ALU.add), ["tot", "ones"], ["cum"])
    NCH = CAP // SCH
    cntf = p.sbuf("cntf", [128, 4]); flf = p.sbuf("flf", [128, 4, NCH]); fli = p.sbuf("fli", [128, 4 * NCH], I32)
    cp(p, "dve", cntf[:], cum[:, NTT - 1, :], ["cum"], ["cntf"])
    for ch_ in range(NCH):
        ts(p, "dve", flf[:, :, ch_], cntf[:], float(ch_ * SCH), ALU.is_gt, ["cntf"], ["flf"])
    cp(p, "dve", fli[:], flf[:].rearrange("p a b -> p (a b)"), ["flf"], ["fli"])
    tt(p, "dve", cum[:], cum[:], tot[:], ALU.subtract, ["cum", "tot"], ["cum"])
    tt(p, "dve", rank[:].rearrange("p a b -> p (a b)"), P[0][:, 0:NTT * 4], cum[:].rearrange("p a b -> p (a b)"), ALU.add, [("ps", 0), "cum"], ["rank"])
    stt(p, "dve", rank[:], rank[:], 1.0, msk[:], ALU.add, ALU.mult, ["rank", "msk"], ["rank"])
    ts(p, "dve", rank[:], rank[:], -1.0, ALU.add, ["rank"], ["rank"])
    tokc = p.sbuf("tokc", [128, NTT, 4, 8], BF16)
    tilev = p.sbuf("tilev", [128, NTT])
    rwh = p.sbuf("rwh", [128, NTT, 4], BF16); rwh32 = p.sbuf("rwh32", [128, NTT, 4]); rwl = p.sbuf("rwl", [128, NTT, 4])
    p.op("pool", lambda e: nc.gpsimd.iota(tilev[:], pattern=[[1, NTT]], base=0, channel_multiplier=0, allow_small_or_imprecise_dtypes=True), [], ["tilev"])
    memset(p, "dve", tokc[:], 0.0, ["tokc"])
    cp(p, "dve", rwh[:], rw[:], ["rw"], ["rwh"])
    cp(p, "dve", rwh32[:], rwh[:], ["rwh"], ["rwh32"])
    tt(p, "dve", rwl[:], rw[:], rwh32[:], ALU.subtract, ["rw", "rwh32"], ["rwl"])
    for e in range(4):
        cp(p, "dve", tokc[:, :, e, 0], tilev[:], ["tilev", "tokc"], ["tokc"])
        cp(p, "dve", tokc[:, :, e, 1], pc[:, 0:1].to_broadcast([128, NTT]), ["pc_id", "tokc"], ["tokc"])
        cp(p, "dve", tokc[:, :, e, 2], ones[:, 0:NTT], ["ones", "tokc"], ["tokc"])
        cp(p, "dve", tokc[:, :, e, 3], rwh[:, :, e], ["rwh", "tokc"], ["tokc"])
        cp(p, "dve", tokc[:, :, e, 4], rwl[:, :, e], ["rwl", "tokc"], ["tokc"])
    OHH = CAP // 2
    iot = p.sbuf("iot", [128, OHH])
    p.op("pool", lambda e: nc.gpsimd.iota(iot[:], pattern=[[1, OHH]], base=0, channel_multiplier=0, allow_small_or_imprecise_dtypes=True), [], ["iot"])
    rank2 = p.sbuf("rank2", [128, NTT, 4])
    ts(p, "dve", rank2[:], rank[:], -float(OHH), ALU.add, ["rank"], ["rank2"])
    _oh = p.sbuf("OH0", [128, CAP], BF16)
    OH = [_oh, _oh]
    EC = NB * 8
    zl = p.sbuf("zl", [128, 128], BF16); zr = p.sbuf("zr", [128, 2 * EC], BF16)
    memset(p, "dve", zl[:], 0.0, ["zl"]); memset(p, "dve", zr[:], 0.0, ["zr"])
    for bk_ in (6, 7):
        mm(p, P[bk_][:, 0:2 * EC], zl[:], zr[:], True, False, ["zl", "zr"], [("ps", bk_)])
    n = 0
    for t in range(NTT):
        for e in range(4):
            oh = OH[0]
            ts(p, "dve", oh[:, 0:OHH], iot[:], rank[:, t, e:e + 1], ALU.is_equal, ["iot", "rank"], [("OH", 0)])
            ts(p, "pool", oh[:, OHH:CAP], iot[:], rank2[:, t, e:e + 1], ALU.is_equal, ["iot", "rank2"], [("OH", 1)])
            for b in range(NB):
                c0 = (e % 2) * EC + b * 8
                mm(p, P[6 + e // 2][:, c0:c0 + 5], oh[:, b * 128:(b + 1) * 128], tokc[:, t, e, 0:5], False, (t == NTT - 1 and b == NB - 1),
                   [("OH", 0), ("OH", 1), "tokc"], [("ps", 6 + e // 2)])
            n += 1
    sacc = p.sbuf("sacc", [128, 4, NB, 8])
    for hh_ in range(2):
        cp(p, "dve", sacc[:, 2 * hh_:2 * hh_ + 2, :, :].rearrange("p a b c -> p (a b c)"), P[6 + hh_][:, 0:2 * EC], [("ps", 6 + hh_)], ["sacc"])
    padv = p.sbuf("padv", [128, NB])
    p.op("pool", lambda e: nc.gpsimd.iota(padv[:], pattern=[[128, NB]], base=NTOK, channel_multiplier=1, allow_small_or_imprecise_dtypes=True), [], ["padv"])
    idxf = p.sbuf("idxf", [128, 4, NB]); t2 = p.sbuf("t2", [128, 4, NB]); idx = p.sbuf("idx", [128, 4, NB], I32); wsl = p.sbuf("wsl", [128, 4, NB])
    for e in range(4):
        stt(p, "dve", idxf[:, e, :], sacc[:, e, :, 0], 128.0, sacc[:, e, :, 1], ALU.mult, ALU.add, ["sacc"], ["idxf"])
        tt(p, "dve", t2[:, e, :], sacc[:, e, :, 2], padv[:], ALU.mult, ["sacc", "padv"], ["t2"])
        tt(p, "dve", t2[:, e, :], padv[:], t2[:, e, :], ALU.subtract, ["padv", "t2"], ["t2"])
        tt(p, "dve", idxf[:, e, :], idxf[:, e, :], t2[:, e, :], ALU.add, ["idxf", "t2"], ["idxf"])
        tt(p, "dve", wsl[:, e, :], sacc[:, e, :, 3], sacc[:, e, :, 4], ALU.add, ["sacc"], ["wsl"])
    cp(p, "dve", idx[:], idxf[:], ["idxf"], ["idx"])

    xbT = p.sbuf("xbT", [128, KC, SCH], BF16)
    actT = p.sbuf("actT", [128, KC, SCH], BF16)
    xg = [p.sbuf("xg%d" % i, [128, D]) for i in range(2)]
    wst = [p.sbuf("wst%d" % i, [128, KC, 256]) for i in range(2)]
    wbf = [[p.sbuf("wbf%d_%d" % (i, j), [128, KC, 256], BF16) for j in range(2)] for i in range(2)]
    bupT = p.sbuf("bupT", [128, 4, 32])
    p.dma("sp", bupT[:].rearrange("p a b -> p (a b)"), bupT_d, writes=["bupT"])
    bdr = p.sbuf("bdr", [1, D], BF16); ones1 = p.sbuf("ones1", [1, 128], BF16)
    memset(p, "dve", ones1[:], 1.0, ["ones1"])
    G = {nm: [p.sbuf("g_%s%d" % (nm, i), [128, 512]) for i in range(2)] for nm in ("g", "sg", "u")}
    NY = 3
    ybs = [p.sbuf("ybs%d" % i, [128, 512]) for i in range(NY)]
    ng = 0; ne = 0; ny = 0; nwu = 0
    for e in range(4):
        p.dma("pool", bdr[:], bdn_d[e:e + 1, :], writes=["bdr"])
        for ch in range(CAP // SCH):
            if ch > 0:
                p.region_begin(fli[0:1, e * NCH + ch:e * NCH + ch + 1], ["fli"])
            for bl in range(NBC):
                b = ch * NBC + bl
                xs = xg[ng % 2]
                p.op("pool", lambda e_, xs=xs, e=e, b=b: nc.gpsimd.indirect_dma_start(
                    out=xs[:], out_offset=None, in_=h2p, in_offset=bass.IndirectOffsetOnAxis(ap=idx[:, e, b:b + 1], axis=0)),
                    ["idx"], [("xg", ng % 2)], dma=True, key=("dma", ("xg", ng % 2)))
                for kq in range(4):
                    bk = kq % 2
                    for j in range(4):
                        k = kq * 4 + j
                        tr(p, P[bk][:, j * 128:(j + 1) * 128], xs[:, k * 128:(k + 1) * 128], ident[:], [("xg", ng % 2)], [("ps", bk)])
                    cp(p, "act" if kq % 2 else "dve", xbT[:, kq * 4:(kq + 1) * 4, bl * 128:(bl + 1) * 128],
                       P[bk][:].rearrange("p (j s) -> p j s", j=4), [("ps", bk)], [("xbT", bl)])
                ng += 1
            allx = [("xbT", bl) for bl in range(NBC)]
            for fq in range(8):
                par = nwu % 2
                for hf in range(2):
                    c0 = hf * D + fq * 256
                    p.dma("sp", wst[hf][:], wup_d[e, :, c0:c0 + 256].rearrange("(k p) c -> p k c", p=128), writes=[("wst", hf)])
                    cp(p, "dve", wbf[hf][par][:], wst[hf][:], [("wst", hf)], [("wbf", hf, par)])
                nwu += 1
                for j in range(2):
                    fc = fq * 2 + j
                    for bi, (s0, sn) in enumerate(SBLK):
                        bg = 2 + (ne % 2) * 2; bu = bg + 1
                        for k in range(KC):
                            mm(p, P[bg][:, 0:sn], wbf[0][par][:, k, j * 128:(j + 1) * 128], xbT[:, k, s0:s0 + sn], k == 0, k == KC - 1, [("wbf", 0, par)] + allx, [("ps", bg)])
                        for k in range(KC):
                            mm(p, P[bu][:, 0:sn], wbf[1][par][:, k, j * 128:(j + 1) * 128], xbT[:, k, s0:s0 + sn], k == 0, k == KC - 1, [("wbf", 1, par)] + allx, [("ps", bu)])
                        w_ = ne % 2
                        g_, sg_, u_ = G["g"][w_][:, 0:sn], G["sg"][w_][:, 0:sn], G["u"][w_][:, 0:sn]
                        ts(p, "dve", g_, P[bg][:, 0:sn], bupT[:, e, fc:fc + 1], ALU.add, [("ps", bg), "bupT"], [("g", w_)], s2=SW_LIMIT, op1=ALU.min)
                        act(p, sg_, g_, AF.Sigmoid, [("g", w_)], [("sg", w_)], scale=SW_ALPHA)
                        ts(p, "dve", u_, P[bu][:, 0:sn], bupT[:, e, 16 + fc:17 + fc], ALU.add, [("ps", bu), "bupT"], [("u", w_)], s2=SW_LIMIT, op1=ALU.min)
                        ts(p, "dve", u_, u_, -SW_LIMIT, ALU.max, [("u", w_)], [("u", w_)], s2=1.0, op1=ALU.add)
                        tt(p, "dve", g_, g_, sg_, ALU.mult, [("g", w_), ("sg", w_)], [("g", w_)])
                        tt(p, "dve", actT[:, fc, s0:s0 + sn], g_, u_, ALU.mult, [("g", w_), ("u", w_)], [("actT", bi)])
                        ne += 1
            alla = [("actT", bi) for bi in range(len(SBLK))]
            for db in range(4):
                par = nwu % 2
                for hf in range(2):
                    c0 = db * 512 + hf * 256
                    p.dma("sp", wst[hf][:], wdn_d[e, :, c0:c0 + 256].rearrange("(k p) c -> p k c", p=128), writes=[("wst", hf)])
                    cp(p, "dve", wbf[hf][par][:], wst[hf][:], [("wst", hf)], [("wbf", hf, par)])
                nwu += 1
                for bl in range(NBC):
                    b = ch * NBC + bl
                    bk = ny % 2
                    for hf in range(2):
                        c0 = db * 512 + hf * 256
                        mm(p, P[bk][:, hf * 256:(hf + 1) * 256], ones1[:], bdr[:, c0:c0 + 256], True, False, ["ones1", "bdr"], [("ps", bk)])
                        for k in range(KC):
                            mm(p, P[bk][:, hf * 256:(hf + 1) * 256], actT[:, k, bl * 128:(bl + 1) * 128], wbf[hf][par][:, k, :], False, k == KC - 1,
                               [("wbf", hf, par)] + alla, [("ps", bk)])
                    yi = ny % NY
                    y_ = ybs[yi]
                    act(p, y_[:], P[bk][:], AF.Copy, [("ps", bk), "wsl"], [("ybs", yi)], scale=wsl[:, e, b:b + 1])
                    rd = [("ybs", yi), "idx"]
                    if e == 0:
                        rd += yz_keys
                    else:
                        rd += [("ya", db, e - 1, b2) for b2 in range(NB)]
                    p.op("pool", lambda e_, y_=y_, e=e, b=b, db=db: nc.gpsimd.indirect_dma_start(
                        out=yp_o[db], out_offset=bass.IndirectOffsetOnAxis(ap=idx[:, e, b:b + 1], axis=0), in_=y_[:], in_offset=None,
                        compute_op=ALU.add), rd, [("ya", db, e, b)], dma=True, key=("dma", ("yacc", db)))
                    ny += 1
            if ch > 0:
                p.region_end()
    p.emit()
    return p


def build_comb(nc):
    p = Prog(nc)
    x1_d = dram_in(nc, "x1", [TL, D])
    yp_d = dram_in(nc, "yps", [NCORES, TL, D])
    c2T = dram_in(nc, "c2T", [128, 32])
    wmod = dram_in(nc, "wmod", [D, D])
    bmodT = dram_in(nc, "bmodT", [128, KC])
    fg_d = dram_in(nc, "fg", [1, D])
    x2_o = dram_out(nc, "x2", [TL, D])
    xn_o = dram_out(nc, "xn", [TL, D])
    P = [p.psum("ps%d" % i, [128, 512]) for i in range(8)]
    ident, io, pc = make_ident(p)
    modT = adaln_T(p, P, c2T, wmod, bmodT, 16)
    ones = p.sbuf("ones", [128, 128])
    memset(p, "dve", ones[:], 1.0, ["ones"])
    g2b = p.sbuf("g2b", [128, 2, D])
    dg = [p.sbuf("dg%d" % i, [128, 128]) for i in range(2)]
    for r in range(2):
        for dc in range(KC):
            s = dc % 2
            ts(p, "dve", dg[s][:], ident[:], modT[:, dc, r:r + 1], ALU.mult, ["ident", "modT"], [("dg", s)])
            b = (dc // 4) % 2
            mm(p, P[b][:, (dc % 4) * 128:(dc % 4 + 1) * 128], ones[:], dg[s][:], True, True, ["ones", ("dg", s)], [("ps", b)])
            if dc % 4 == 3:
                cp(p, "act", g2b[:, r, (dc // 4) * 512:(dc // 4 + 1) * 512], P[b][:], [("ps", b)], ["g2b"])
    fgb = p.sbuf("fgb", [128, D])
    p.dma("pool", fgb[:], fg_d.partition_broadcast(128), writes=["fgb"])
    xb = [p.sbuf("xb%d" % i, [128, D]) for i in range(2)]
    yb = [p.sbuf("yb%d" % i, [128, 4, D]) for i in range(2)]
    xo = [p.sbuf("xo%d" % i, [128, D]) for i in range(2)]
    junk = p.sbuf("junk", [128, D], BF16)
    ssq = p.sbuf("ssq", [128, NT]); rs = p.sbuf("rs", [128, NT])
    for t in range(NT):
        r = 0 if t < 8 else 1
        s = t % 2
        rows = slice(t * 128, (t + 1) * 128)
        p.dma("sp", xb[s][:], x1_d[rows, :], writes=[("xb", s)])
        for hv in range(2):
            p.dma("sp", yb[hv][:], yp_d[hv * 4:(hv + 1) * 4, rows, :].rearrange("c p d -> p c d"), writes=[("yb", hv)])
        tt(p, "dve", yb[0][:, 0:2, :], yb[0][:, 0:2, :], yb[0][:, 2:4, :], ALU.add, [("yb", 0)], [("yb", 0)])
        tt(p, "pool", yb[1][:, 0:2, :], yb[1][:, 0:2, :], yb[1][:, 2:4, :], ALU.add, [("yb", 1)], [("yb", 1)])
        tt(p, "dve", yb[0][:, 0:2, :], yb[0][:, 0:2, :], yb[1][:, 0:2, :], ALU.add, [("yb", 0), ("yb", 1)], [("yb", 0)])
        tt(p, "pool", yb[0][:, 0, :], yb[0][:, 0, :], yb[0][:, 1, :], ALU.add, [("yb", 0)], [("yb", 0)])
        tt(p, "dve", yb[0][:, 0, :], yb[0][:, 0, :], g2b[:, r, :], ALU.mult, [("yb", 0), "g2b"], [("yb", 0)])
        tt(p, "pool", xb[s][:], xb[s][:], yb[0][:, 0, :], ALU.add, [("yb", 0), ("xb", s)], [("xb", s)])
        p.dma("pool", x2_o[rows, :], xb[s][:], reads=[("xb", s)], key=("x2o", s))
        act(p, junk[:], xb[s][:], AF.Square, [("xb", s)], ["junk", ("ssq", t)], accum_out=ssq[:, t:t + 1])
        act(p, rs[:, t:t + 1], ssq[:, t:t + 1], AF.Sqrt, [("ssq", t)], [("rs", t)], scale=1.0 / D, bias=EPS)
        recip(p, rs[:, t:t + 1], rs[:, t:t + 1], [("rs", t)], [("rs", t)])
        stt(p, "dve", xo[s][:], xb[s][:], rs[:, t:t + 1], fgb[:], ALU.mult, ALU.mult, [("xb", s), ("rs", t), "fgb"], [("xo", s)])
        p.dma("pool", xn_o[rows, :], xo[s][:], reads=[("xo", s)], key=("xno", s))
    p.emit()
    return p


def fm(v, nch):
    return np.ascontiguousarray(np.asarray(v).reshape(nch, 128).T)

def c2T_of(c, c_ctx):
    c2 = np.stack([np.asarray(c).reshape(-1), np.asarray(c_ctx).reshape(-1)])
    return np.ascontiguousarray(c2.reshape(2, 16, 128).transpose(2, 1, 0).reshape(128, 32))


def s5_core_inputs(cid, are, aim, ldt, bre, bim, cre, cim):
    spar = np.zeros((128, 4, 3), np.float32)
    bre_b = np.zeros((128, 4, 32), np.float32); bim_b = np.zeros((128, 4, 32), np.float32)
    cre_b = np.zeros((128, 4, 64), np.float32); cim_b = np.zeros((128, 4, 64), np.float32)
    for d in range(2):
        for gp in range(2):
            ti = d * 2 + gp
            for g2 in range(2):
                G = 4 * cid + gp * 2 + g2
                ps = slice(g2 * 64, g2 * 64 + 64)
                spar[ps, ti, 0] = are[d, G]; spar[ps, ti, 1] = aim[d, G]; spar[ps, ti, 2] = ldt[d, G]
                bre_b[ps, ti, g2 * 16:(g2 + 1) * 16] = bre[d, G]
                bim_b[ps, ti, g2 * 16:(g2 + 1) * 16] = bim[d, G]
                c0 = gp * 32 + g2 * 16
                cre_b[ps, ti, c0:c0 + 16] = cre[d, G].T
                cim_b[ps, ti, c0:c0 + 16] = cim[d, G].T
    return dict(spar=spar, bre=bre_b, bim=bim_b, cre=cre_b, cim=cim_b)


def mix_inputs(cid, zs5T_lat, zs5T_ctx, zfT_lat, zfT_ctx, s5p):
    rows = slice(64 * cid, 64 * cid + 64)
    uf = np.concatenate([zs5T_ctx[rows], zs5T_lat[rows]], 1)
    ub = np.concatenate([zs5T_ctx[rows][:, ::-1], zs5T_lat[rows][:, ::-1]], 1)
    g, half = cid // 2, cid % 2
    kbv = np.tile(np.arange(8, dtype=np.float32)[None] + 8 * half, (128, 1)).astype(np.float32)
    d = dict(uf=np.ascontiguousarray(uf), ub=np.ascontiguousarray(ub),
             zfl=np.ascontiguousarray(zfT_lat[g * 128:(g + 1) * 128]), zfc=np.ascontiguousarray(zfT_ctx[g * 128:(g + 1) * 128]), kbv=kbv)
    d.update(s5_core_inputs(cid, *s5p))
    return d


def _run(build, in_maps):
    nc = bass.Bass("TRN2", target_bir_lowering=False)
    build(nc)
    res = run_bass_kernel_spmd(nc, in_maps, core_ids=list(range(NCORES)))
    return res.results


def _rows_from_T(hT):
    a = np.asarray(hT)
    return np.ascontiguousarray(a.transpose(2, 1, 0).reshape(a.shape[2], a.shape[1] * 128))


def kernel(x, c, ctx, c_ctx, w_mod, b_mod, norm1_g, norm2_g, w_in,
           s5_a_re, s5_a_im, s5_log_dt, s5_b_re, s5_b_im, s5_c_re, s5_c_im, s5_d, s5_w_glu,
           sgu_norm_g, sgu_w, sgu_b, w_branch, w_gate, b_gate, w_out,
           router_w, router_b, moe_w_up, moe_b_up, moe_w_down, moe_b_down, final_g):
    f32 = np.float32
    A = lambda a: np.ascontiguousarray(np.asarray(a, dtype=f32))
    xl = np.asarray(x, dtype=f32)[0]
    xc = np.asarray(ctx, dtype=f32)[0]
    c2T = c2T_of(np.asarray(c, dtype=f32), np.asarray(c_ctx, dtype=f32))
    xs = [np.concatenate([xl[TLAT * j:TLAT * (j + 1)], xc], 0) for j in range(NCORES)]
    depth = np.asarray(w_mod).shape[0]
    xn = None
    for L in range(depth):
        wm = np.asarray(w_mod[L], dtype=f32); bm = np.asarray(b_mod[L], dtype=f32)
        shared = dict(c2T=c2T, wmod=A(wm[:, :4096]), bmodT=fm(bm[:4096], 32), g1nT=fm(norm1_g[L], KC), w_in=A(w_in[L]),
                      gv=A(sgu_norm_g[L]).reshape(1, 512), sguw=A(sgu_w[L]), sgub=A(sgu_b[L]).reshape(1, 512))
        pre = _run(build_pre, [dict(shared, x=xs[j]) for j in range(NCORES)])
        zT = [np.asarray(pre[j]["zT"]) for j in range(NCORES)]
        zs5_lat = np.concatenate([zT[j][0:512, :TLAT] for j in range(NCORES)], 1)
        zf_lat = np.concatenate([zT[j][512:1024, :TLAT] for j in range(NCORES)], 1)
        zs5_ctx = zT[0][0:512, TLAT:]
        zf_ctx = zT[0][512:1024, TLAT:]
        s5p = [np.asarray(a[L], dtype=f32) for a in (s5_a_re, s5_a_im, s5_log_dt, s5_b_re, s5_b_im, s5_c_re, s5_c_im)]
        mix = _run(build_mix, [mix_inputs(j, zs5_lat, zs5_ctx, zf_lat, zf_ctx, s5p) for j in range(NCORES)])
        ysf_seq = np.concatenate([np.asarray(mix[j]["ysf"]) for j in range(NCORES)], 0)
        ysb_seq = np.concatenate([np.asarray(mix[j]["ysb"]) for j in range(NCORES)], 0)
        ysf_ctx, ysf_lat = ysf_seq[:, :CTX], ysf_seq[:, CTX:]
        ysb_ctx, ysb_lat = ysb_seq[:, :CTX][:, ::-1], ysb_seq[:, CTX:][:, ::-1]
        yf_lat = np.concatenate([np.concatenate([np.asarray(mix[2 * g + h]["yf"]) for h in range(2)], 1) for g in range(4)], 0)
        yf_ctx = np.concatenate([np.asarray(mix[2 * g]["yfc"]) for g in range(4)], 0)

        def tokT(lat, cx, j):
            return np.ascontiguousarray(np.concatenate([lat[:, TLAT * j:TLAT * (j + 1)], cx], 1))
        sh1 = dict(s5d=fm(s5_d[L], 4), wglu=A(s5_w_glu[L]), wbr=A(w_branch[L]), wgate=A(w_gate[L]), bgT=fm(b_gate[L], 48))
        p1 = _run(build_posta1, [dict(sh1, hT=np.asarray(pre[j]["hT"]), uT=np.ascontiguousarray(zT[j][0:512]),
                                     ysf=tokT(ysf_lat, ysf_ctx, j), ysb=tokT(ysb_lat, ysb_ctx, j), yf=tokT(yf_lat, yf_ctx, j),
                                     ysgu=np.asarray(pre[j]["ysgu"])) for j in range(NCORES)])
        sh2 = dict(c2T=c2T, wmod=A(wm[:, 4096:10240]), bmodT=fm(bm[4096:10240], 48), g2nT=fm(norm2_g[L], KC), wout=A(w_out[L]),
                   rw=A(router_w[L]), rb=A(router_b[L]).reshape(1, 32))
        p2 = _run(build_posta2, [dict(sh2, x=xs[j], mT=np.asarray(p1[j]["mT"])) for j in range(NCORES)])
        h2r = [_rows_from_T(p2[j]["h2T"]) for j in range(NCORES)]
        h2p = np.concatenate([h2r[j][:TLAT] for j in range(NCORES)] + [h2r[0][TLAT:], np.zeros((CAP, D), f32)], 0)
        rwa = np.concatenate([np.asarray(p2[j]["rwt"])[:TLAT] for j in range(NCORES)] + [np.asarray(p2[0]["rwt"])[TLAT:]], 0)
        moe_in = []
        for j in range(NCORES):
            es = slice(4 * j, 4 * j + 4)
            rwc = rwa[:, es]
            rwT = np.ascontiguousarray(rwc.reshape(NTT, 128, 4).transpose(1, 0, 2).reshape(128, NTT * 4))
            bup = np.asarray(moe_b_up[L][es], dtype=f32)
            bupT = np.ascontiguousarray(bup.reshape(4, 32, 128).transpose(2, 0, 1).reshape(128, 128))
            moe_in.append(dict(h2p=h2p, rwT=rwT, wup=A(moe_w_up[L][es]), bupT=bupT, wdn=A(moe_w_down[L][es]), bdn=A(moe_b_down[L][es])))
        mo = _run(build_moe, moe_in)
        del moe_in
        yparts = [np.concatenate([np.asarray(mo[j]["yp%d" % i]) for i in range(4)], 1) for j in range(NCORES)]
        shc = dict(c2T=c2T, wmod=A(wm[:, 10240:12288]), bmodT=fm(bm[10240:12288], KC), fg=A(final_g).reshape(1, D))
        cin = []
        for j in range(NCORES):
            yps = np.stack([np.concatenate([yparts[e][TLAT * j:TLAT * (j + 1)], yparts[e][SEQ:NTOK]], 0) for e in range(NCORES)], 0)
            cin.append(dict(shc, x1=np.asarray(p2[j]["x1"]), yps=yps))
        cb = _run(build_comb, cin)
        del cin
        xs = [np.asarray(cb[j]["x2"]) for j in range(NCORES)]
        xn = [np.asarray(cb[j]["xn"]) for j in range(NCORES)]
    out = np.concatenate([xn[j][:TLAT] for j in range(NCORES)], 0).reshape(1, SEQ, D).astype(np.float32)
    return out
```
